# Optimizing a Trainium2 kernel written in Bass

```python
import jax, jax.numpy as jnp
from jax import lax
import numpy as np

D_MODEL = 1024
BATCH = 8
SEQ = 2048
DEPTH = 4

RET_HEADS = 4
RET_DK = 128
RET_DV = 256
RET_QK = RET_HEADS * RET_DK
RET_V = RET_HEADS * RET_DV
ROPE_BASE = 10000.0
MAX_POS_OFFSET = 1024
LRU_WIDTH = D_MODEL
LRU_BLOCKS = 16
LRU_BLOCK = LRU_WIDTH // LRU_BLOCKS
LRU_C = 8.0
CONV_W = 4
MLSTM_HEADS = 4
MLSTM_WIDTH = D_MODEL
MLSTM_DH = MLSTM_WIDTH // MLSTM_HEADS
QKV_BLOCK = 4
CHUNK = 64
D_FF = 4 * D_MODEL
N_BRANCH = 3
EPS = 1e-6

IN_SPLITS = (RET_QK, RET_QK, RET_V, RET_V,
             LRU_WIDTH, LRU_WIDTH,
             MLSTM_WIDTH, MLSTM_WIDTH,
             N_BRANCH * D_MODEL)
D_IN = int(sum(IN_SPLITS))
IN_OFFSETS = tuple(int(v) for v in np.cumsum(IN_SPLITS)[:-1])

kernel_name = "hybrid_retention_rglru_mlstm_trunk"


def rms_norm(x, gain):
    xf = x.astype(jnp.float32)
    xf = xf * lax.rsqrt(jnp.mean(xf * xf, axis=-1, keepdims=True) + EPS)
    return xf.astype(x.dtype) * gain


def head_rms_norm(xh):
    xf = xh.astype(jnp.float32)
    xf = xf * lax.rsqrt(jnp.mean(xf * xf, axis=-1, keepdims=True) + EPS)
    return xf.astype(xh.dtype)


def causal_depthwise_conv(x, w, b):
    S = x.shape[1]
    xp = jnp.pad(x, ((0, 0), (CONV_W - 1, 0), (0, 0)))
    y = b
    for k in range(CONV_W):
        y = y + w[k] * xp[:, k:k + S]
    return y


def block_diag_linear(x, w):
    B, S, _ = x.shape
    nb, bs, bo = w.shape
    return jnp.einsum('bsnd,nde->bsne', x.reshape(B, S, nb, bs), w).reshape(B, S, nb * bo)


def rotary(x, positions):
    half = x.shape[-1] // 2
    inv_freq = ROPE_BASE ** (-jnp.arange(half, dtype=jnp.float32) / half)
    ang = positions.astype(jnp.float32)[..., None] * inv_freq
    cos = jnp.cos(ang)[:, :, None, :].astype(x.dtype)
    sin = jnp.sin(ang)[:, :, None, :].astype(x.dtype)
    x1, x2 = x[..., :half], x[..., half:]
    return jnp.concatenate([x1 * cos - x2 * sin, x1 * sin + x2 * cos], axis=-1)


def to_chunks(x):
    B, S, H, d = x.shape
    return x.reshape(B, S // CHUNK, CHUNK, H, d).transpose(1, 0, 3, 2, 4)


def gate_to_chunks(g):
    B, S, H = g.shape
    return g.reshape(B, S // CHUNK, CHUNK, H).transpose(1,0, 3, 2)


def from_chunks(y):
    N, B, H, C, d = y.shape
    return y.transpose(1, 0, 3, 2, 4).reshape(B, N * C, H, d)


def chunked_retention(q, k, v):
    dtype = v.dtype
    q, k, v = q.astype(jnp.float32), k.astype(jnp.float32) * RET_DK ** -0.5, v.astype(jnp.float32)
    B = q.shape[0]
    log_gamma = jnp.log1p(-jnp.exp2(-5.0 - jnp.arange(RET_HEADS, dtype=jnp.float32)))
    pos = jnp.arange(CHUNK, dtype=jnp.float32)
    diff = pos[:, None] - pos[None, :]
    causal = diff >= 0
    decay_intra = jnp.where(causal, jnp.exp(log_gamma[:, None, None] * jnp.where(causal, diff, 0.0)), 0.0)
    decay_q = jnp.exp(log_gamma[:, None] * (pos + 1.0))[:, :, None]
    decay_k = jnp.exp(log_gamma[:, None] * (CHUNK - 1.0 - pos))[:, :, None]
    decay_chunk = jnp.exp(log_gamma * CHUNK)[:, None, None]

    def step(state, qkv):
        qc, kc, vc = qkv
        scores = jnp.einsum('bhid,bhjd->bhij', qc, kc) * decay_intra
        out = (jnp.einsum('bhij,bhje->bhie', scores, vc)
               + jnp.einsum('bhid,bhde->bhie', qc * decay_q, state))
        state = decay_chunk * state + jnp.einsum('bhjd,bhje->bhde', kc * decay_k, vc)
        return state, out

    state0 = jnp.zeros((B, RET_HEADS, RET_DK, RET_DV), jnp.float32)
    _, out = lax.scan(step, state0, (to_chunks(q), to_chunks(k), to_chunks(v)))
    return from_chunks(out).astype(dtype)


def rg_lru(x, w_r, b_r, w_i, b_i, lam):
    dtype = x.dtype
    xf = x.astype(jnp.float32)
    r = jax.nn.sigmoid(block_diag_linear(xf, w_r.astype(jnp.float32)) + b_r)
    i = jax.nn.sigmoid(block_diag_linear(xf, w_i.astype(jnp.float32)) + b_i)
    log_a = -LRU_C * r * jax.nn.softplus(-lam.astype(jnp.float32))
    a = jnp.exp(log_a)
    u = jnp.sqrt(-jnp.expm1(2.0 * log_a)) * (i * xf)

    def combine(left, right):
        a1, b1 = left
        a2, b2 = right
        return a1 * a2, a2 * b1 + b2

    _, h = lax.associative_scan(combine, (a, u), axis=1)
    return h.astype(dtype)


def chunked_mlstm(q, k, v, i_pre, f_pre):
    dtype = v.dtype
    q, k, v = q.astype(jnp.float32), k.astype(jnp.float32) * MLSTM_DH ** -0.5, v.astype(jnp.float32)
    i_pre = i_pre.astype(jnp.float32)
    log_f = jax.nn.log_sigmoid(f_pre.astype(jnp.float32))
    B = q.shape[0]
    causal = jnp.tril(jnp.ones((CHUNK, CHUNK), dtype=bool))

    def step(carry, inp):
        C_s, n_s, m_s = carry
        qc, kc, vc, ic, lfc = inp
        b = jnp.cumsum(lfc, axis=-1)
        D = jnp.where(causal, b[..., :, None] - b[..., None, :] + ic[..., None, :], -jnp.inf)
        inter = b + m_s[..., None]
        m_t = jnp.maximum(inter, jnp.max(D, axis=-1))
        w_inter = jnp.exp(inter - m_t)
        s = jnp.einsum('bhid,bhjd->bhij', qc, kc) * jnp.exp(D - m_t[..., None])
        num = (jnp.einsum('bhij,bhje->bhie', s, vc)
               + w_inter[..., None] * jnp.einsum('bhid,bhde->bhie', qc, C_s))
        den = jnp.sum(s, axis=-1) + w_inter * jnp.einsum('bhid,bhd->bhi', qc, n_s)
        h = num / jnp.maximum(jnp.abs(den), jnp.exp(-m_t))[..., None]
        b_last = b[..., -1]
        m_new = m_t[..., -1]
        w_k = jnp.exp(b_last[..., None] - b + ic - m_new[..., None])
        decay = jnp.exp(b_last + m_s - m_new)
        C_new = decay[..., None, None] * C_s + jnp.einsum('bhjd,bhje->bhde', kc * w_k[..., None], vc)
        n_new = decay[..., None] * n_s + jnp.einsum('bhjd,bhj->bhd', kc, w_k)
        return (C_new, n_new, m_new), h

    carry0 = (jnp.zeros((B, MLSTM_HEADS, MLSTM_DH, MLSTM_DH), jnp.float32),
              jnp.zeros((B, MLSTM_HEADS, MLSTM_DH), jnp.float32),
              jnp.zeros((B, MLSTM_HEADS), jnp.float32))
    _, h = lax.scan(step, carry0, (to_chunks(q), to_chunks(k), to_chunks(v),
                                   gate_to_chunks(i_pre), gate_to_chunks(log_f)))
    return from_chunks(h).astype(dtype)


def hybrid_mixer(h, positions, w_in, lru_conv_w, lru_conv_b, lru_w_r, lru_b_r, lru_w_i, lru_b_i,
                 lru_lambda, m_conv_w, m_conv_b, m_w_q, m_w_k, m_w_v, m_w_if, m_b_if, m_norm,
                 w_br_ret, w_br_lru, w_br_mlstm, w_out):
    B, S, _ = h.shape
    proj = h @ w_in
    rq, rk, rv, rg, lx, ly, mx, mo, gate_pre = jnp.split(proj, IN_OFFSETS, axis=-1)

    q = rotary(rq.reshape(B, S, RET_HEADS, RET_DK), positions)
    k = rotary(rk.reshape(B, S, RET_HEADS, RET_DK), positions)
    v = rv.reshape(B, S, RET_HEADS, RET_DV)
    ret = head_rms_norm(chunked_retention(q, k, v)).reshape(B, S, RET_V) * jax.nn.silu(rg)

    xl = causal_depthwise_conv(lx, lru_conv_w, lru_conv_b)
    lru = rg_lru(xl, lru_w_r, lru_b_r, lru_w_i, lru_b_i, lru_lambda) * jax.nn.gelu(ly)

    xc = jax.nn.silu(causal_depthwise_conv(mx, m_conv_w, m_conv_b))
    mq = block_diag_linear(xc, m_w_q)
    mk = block_diag_linear(xc, m_w_k)
    mv = block_diag_linear(mx, m_w_v)
    if_pre = jnp.concatenate([mq, mk, mv], axis=-1) @ m_w_if + m_b_if
    i_pre, f_pre = jnp.split(if_pre, 2, axis=-1)
    hm = chunked_mlstm(mq.reshape(B, S, MLSTM_HEADS, MLSTM_DH), mk.reshape(B, S, MLSTM_HEADS, MLSTM_DH),
                       mv.reshape(B, S, MLSTM_HEADS, MLSTM_DH), i_pre, f_pre)
    hm = jax.nn.sigmoid(mo) * hm.reshape(B, S, MLSTM_WIDTH)
    mls = head_rms_norm(hm.reshape(B, S, MLSTM_HEADS, MLSTM_DH)).reshape(B, S, MLSTM_WIDTH) * m_norm

    g_ret, g_lru, g_mls = jnp.split(jax.nn.sigmoid(gate_pre), N_BRANCH, axis=-1)
    merged = g_ret * (ret @ w_br_ret) + g_lru * (lru @ w_br_lru) + g_mls * (mls @ w_br_mlstm)
    return merged @ w_out


def squared_relu_mlp(h, w1, w2):
    return jnp.square(jax.nn.relu(h @ w1)) @ w2


def setup_inputs(seed: int = 0) -> dict:
    key = jax.random.key(seed)
    ks = iter(jax.random.split(key, 40))

    def nrm(shape, scale):
        return scale * jax.random.normal(next(ks), shape, jnp.float32)

    L, D, H = DEPTH, D_MODEL, MLSTM_HEADS
    x = nrm((BATCH, SEQ, D), 1.0)
    c = nrm((BATCH, D), 1.0)
    offset = jax.random.randint(next(ks), (BATCH, 1), 0, MAX_POS_OFFSET, dtype=jnp.int32)
    positions = offset + jnp.arange(SEQ, dtype=jnp.int32)[None, :]
    w_ada = nrm((L, D, 6 * D), 0.5 * D ** -0.5)
    b_ada = nrm((L, 6 * D), 0.02)
    norm_mix = 1.0 + nrm((L, D), 0.02)
    norm_mlp = 1.0 + nrm((L, D), 0.02)
    w_in = nrm((L, D, D_IN), D ** -0.5)
    lru_conv_w = nrm((L, CONV_W, LRU_WIDTH), CONV_W ** -0.5)
    lru_conv_b = nrm((L, LRU_WIDTH), 0.02)
    lru_w_r = nrm((L, LRU_BLOCKS, LRU_BLOCK, LRU_BLOCK), LRU_BLOCK ** -0.5)
    lru_b_r = nrm((L, LRU_WIDTH), 0.1)
    lru_w_i = nrm((L, LRU_BLOCKS, LRU_BLOCK, LRU_BLOCK), LRU_BLOCK ** -0.5)
    lru_b_i = nrm((L, LRU_WIDTH), 0.1)
    a_pow_c = jax.random.uniform(next(ks), (L, LRU_WIDTH), jnp.float32, 0.9, 0.999)
    a_base = a_pow_c ** (1.0 / LRU_C)
    lru_lambda = jnp.log(a_base) - jnp.log1p(-a_base)
    m_conv_w = nrm((L, CONV_W, MLSTM_WIDTH), CONV_W ** -0.5)
    m_conv_b = nrm((L, MLSTM_WIDTH), 0.02)
    nqb = MLSTM_WIDTH // QKV_BLOCK
    m_w_q = nrm((L, nqb, QKV_BLOCK, QKV_BLOCK), QKV_BLOCK ** -0.5)
    m_w_k = nrm((L, nqb, QKV_BLOCK, QKV_BLOCK), QKV_BLOCK ** -0.5)
    m_w_v = nrm((L, nqb, QKV_BLOCK, QKV_BLOCK), QKV_BLOCK ** -0.5)
    m_w_if = nrm((L, 3 * MLSTM_WIDTH, 2 * H), (3 * MLSTM_WIDTH) ** -0.5)
    m_b_if = jnp.concatenate([nrm((L, H), 0.1),
                              jnp.linspace(3.0, 6.0, H, dtype=jnp.float32)[None, :] + nrm((L, H), 0.1)], axis=-1)
    m_norm = 1.0 + nrm((L, MLSTM_WIDTH), 0.02)
    w_br_ret = nrm((L, RET_V, D), RET_V ** -0.5)
    w_br_lru = nrm((L, LRU_WIDTH, D), LRU_WIDTH ** -0.5)
    w_br_mlstm = nrm((L, MLSTM_WIDTH, D), MLSTM_WIDTH ** -0.5)
    w_out = nrm((L, D, D), D ** -0.5)
    w_ff1 = nrm((L, D, D_FF), D ** -0.5)
    w_ff2 = nrm((L, D_FF, D), D_FF ** -0.5)
    final_norm = 1.0 + nrm((D,), 0.02)
    return {"x": x, "c": c, "positions": positions, "w_ada": w_ada, "b_ada": b_ada,
            "norm_mix": norm_mix, "norm_mlp": norm_mlp, "w_in": w_in,
            "lru_conv_w": lru_conv_w, "lru_conv_b": lru_conv_b, "lru_w_r": lru_w_r, "lru_b_r": lru_b_r,
            "lru_w_i": lru_w_i, "lru_b_i": lru_b_i, "lru_lambda": lru_lambda,
            "m_conv_w": m_conv_w, "m_conv_b": m_conv_b, "m_w_q": m_w_q, "m_w_k": m_w_k, "m_w_v": m_w_v,
            "m_w_if": m_w_if, "m_b_if": m_b_if, "m_norm": m_norm,
            "w_br_ret": w_br_ret, "w_br_lru": w_br_lru, "w_br_mlstm": w_br_mlstm, "w_out": w_out,
            "w_ff1": w_ff1, "w_ff2": w_ff2, "final_norm": final_norm}


def reference(x, c, positions, w_ada, b_ada, norm_mix, norm_mlp, w_in,
              lru_conv_w, lru_conv_b, lru_w_r, lru_b_r, lru_w_i, lru_b_i, lru_lambda,
              m_conv_w, m_conv_b, m_w_q, m_w_k, m_w_v, m_w_if, m_b_if, m_norm,
              w_br_ret, w_br_lru, w_br_mlstm, w_out, w_ff1, w_ff2, final_norm):
    cond = jax.nn.silu(c)
    for l in range(DEPTH):
        mod = (cond @ w_ada[l] + b_ada[l])[:, None, :]
        sh1, sc1, g1, sh2, sc2, g2 = jnp.split(mod, 6, axis=-1)
        h = rms_norm(x, norm_mix[l]) * (1.0 + sc1) + sh1
        x = x + g1 * hybrid_mixer(h, positions, w_in[l], lru_conv_w[l], lru_conv_b[l], lru_w_r[l], lru_b_r[l],
                                  lru_w_i[l], lru_b_i[l], lru_lambda[l], m_conv_w[l], m_conv_b[l],
                                  m_w_q[l], m_w_k[l], m_w_v[l], m_w_if[l], m_b_if[l], m_norm[l],
                                  w_br_ret[l], w_br_lru[l], w_br_mlstm[l], w_out[l])
        h = rms_norm(x, norm_mlp[l]) * (1.0 + sc2) + sh2
        x = x + g2 * squared_relu_mlp(h, w_ff1[l], w_ff2[l])
    return rms_norm(x, final_norm)
```

```python
import math
import os
import numpy as np
import concourse.bass as bass
import concourse.mybir as mybir
from concourse.bass_utils import run_bass_kernel_spmd

F32 = mybir.dt.float32
BF16 = mybir.dt.bfloat16
I32 = mybir.dt.int32
AF = mybir.ActivationFunctionType
ALU = mybir.AluOpType
AX = mybir.AxisListType

D = 1024
SEQ = 2048
L = 4
NCH = 8
TB = 512
NTB = 4
D_IN = 10240
EPS = 1e-6
OFF_RQ, OFF_RK, OFF_RV, OFF_RG, OFF_LX, OFF_LY, OFF_MX, OFF_MO, OFF_GATE = 0, 512, 1024, 2048, 3072, 4096, 5120, 6144, 7168
GAMMA = [1.0 - 2.0 ** (-5.0 - h) for h in range(4)]
NB_RING = 4


class Res:
    __slots__ = ("w", "r", "excl", "wn")

    def __init__(self, excl=False):
        self.w = None
        self.wn = 0
        self.r = {}
        self.excl = excl


class Sched:
    ENG = ("pe", "act", "dve", "pool", "sp")
    SAME_N = 256
    STRICT = bool(int(os.environ.get("K_STRICT_SYNC", "0")))
    EPOCH = 8000

    def __init__(self, nc, n_dma_sems=8, record_only=False):
        self.nc = nc
        self.rec = record_only
        self.sems = {}
        self.cnt = {}
        self.ekey = {}
        self.dsem = {}
        self.dcur = {}
        for e in self.ENG:
            self._new_epoch(e, 0)
        for q in ("sp", "pool"):
            self.dsem[q] = ["d_%s%d" % (q, i) for i in range(n_dma_sems)]
            self.dcur[q] = 0
            for k in self.dsem[q]:
                self.cnt[k] = 0
                if not record_only:
                    self.sems[k] = nc.alloc_semaphore("s_" + k)
        self.waited = {e: {} for e in self.ENG}
        self.ops = {e: [] for e in self.ENG}
        self.nops = 0

    def _new_epoch(self, e, idx):
        k = "%s#%d" % (e, idx)
        self.ekey[e] = k
        self.cnt[k] = 0
        if not self.rec:
            self.sems[k] = self.nc.alloc_semaphore("s_%s_%d" % (e, idx))

    def _deps(self, eng, reads, writes):
        need = {}
        pfx = eng + "#"
        for r in reads:
            if r.w is not None:
                k, v = r.w
                if (self.STRICT or not (k.startswith(pfx) and r.wn >= self.SAME_N)) and need.get(k, 0) < v:
                    need[k] = v
            if r.excl:
                for k, v in r.r.items():
                    if not k.startswith(pfx) and need.get(k, 0) < v:
                        need[k] = v
        for r in writes:
            if r.w is not None:
                k, v = r.w
                if (self.STRICT or not k.startswith(pfx)) and need.get(k, 0) < v:
                    need[k] = v
            for k, v in r.r.items():
                if (self.STRICT or not k.startswith(pfx)) and need.get(k, 0) < v:
                    need[k] = v
        out = []
        wd = self.waited[eng]
        for k, v in need.items():
            if eng == "pe" and k.startswith("pe#"):
                continue
            if wd.get(k, 0) < v:
                wd[k] = v
                out.append((k, v))
        return out

    def _mark(self, me, reads, writes, n=0):
        k, v = me
        for r in reads:
            if r.r.get(k, 0) < v:
                r.r[k] = v
        for r in writes:
            r.w = me
            r.wn = n
            r.r = {}

    def op(self, eng, meth, reads=(), writes=(), **kw):
        if self.rec:
            return None
        waits = self._deps(eng, reads, writes)
        k = self.ekey[eng]
        if self.cnt[k] >= self.EPOCH:
            self._new_epoch(eng, int(k.split("#")[1]) + 1)
            k = self.ekey[eng]
        self.cnt[k] += 1
        me = (k, self.cnt[k])
        self.ops[eng].append((waits, meth, kw, (k, 1)))
        o = kw.get("out", kw.get("ap"))
        try:
            n = int(o.free_size()) if o is not None else 0
        except Exception:
            n = 0
        self._mark(me, reads, writes, n)
        self.nops += 1
        return me

    def dma(self, q, out, in_, reads=(), writes=(), **kw):
        if self.rec:
            return None
        lst = self.dsem[q]
        k = lst[self.dcur[q] % len(lst)]
        self.dcur[q] += 1
        waits = self._deps(q, reads, writes)
        if self.cnt[k] > 0 and self.waited[q].get(k, 0) < self.cnt[k]:
            self.waited[q][k] = self.cnt[k]
            waits.append((k, self.cnt[k]))
        self.cnt[k] += 16
        me = (k, self.cnt[k])
        kw = dict(kw)
        kw["out"] = out
        kw["in_"] = in_
        self.ops[q].append((waits, "dma_start", kw, (k, 16)))
        self._mark(me, reads, writes)
        return me

    def barrier(self, engines=("pe", "act", "dve", "sp"), dma=True):
        if self.rec:
            return
        for e in engines:
            if e == "pe":
                continue
            waits = []
            for k in [self.ekey[o] for o in engines if o != e] + (self.dsem["sp"] if dma else []):
                v = self.cnt[k]
                if v > 0 and self.waited[e].get(k, 0) < v:
                    self.waited[e][k] = v
                    waits.append((k, v))
            if waits:
                self.ops[e].append((waits, None, None, None))

    def wait_tokens(self, eng, tokens):
        if self.rec:
            return
        waits = []
        for k, v in tokens:
            if self.waited[eng].get(k, 0) < v:
                self.waited[eng][k] = v
                waits.append((k, v))
        if waits:
            self.ops[eng].append((waits, None, None, None))

    def emit(self):
        sems = self.sems
        ops = self.ops

        def run(e):
            def body(h):
                for waits, meth, kw, inc in ops[e]:
                    for k, v in waits:
                        h.wait_ge(sems[k], v)
                    if meth is not None:
                        ins = getattr(h, meth)(**kw)
                        ins.then_inc(sems[inc[0]], inc[1])
            return body
        with self.nc.Block() as block:
            block.tensor(run("pe"))
            block.scalar(run("act"))
            block.vector(run("dve"))
            block.gpsimd(run("pool"))
            block.sync(run("sp"))


CONST_COLS = {}


def _build_consts():
    cols = []
    off = 0

    def add(name, arr):
        nonlocal off
        a = np.zeros((128, arr.shape[1]), np.float32)
        a[:arr.shape[0]] = arr
        CONST_COLS[name] = (off, arr.shape[1])
        off += arr.shape[1]
        cols.append(a)
    add("ident", np.eye(128, dtype=np.float32))
    p = np.arange(128)
    add("invfreq", (10000.0 ** (-(p % 64).astype(np.float32) / 64.0)).astype(np.float32)[:, None])
    jj = p[:, None].astype(np.float64)
    ii = p[None, :].astype(np.float64)
    for h in range(4):
        m = np.where(ii >= jj, GAMMA[h] ** np.maximum(ii - jj, 0.0), 0.0) / math.sqrt(128.0)
        add("retmask%d" % h, m.astype(np.float32))
    for h in range(4):
        add("dq%d" % h, np.broadcast_to((GAMMA[h] ** (ii + 1.0)), (128, 128)).astype(np.float32))
    kd = np.stack([GAMMA[h] ** (127.0 - p) / math.sqrt(128.0) for h in range(4)], axis=1)
    add("kdscale", kd.astype(np.float32))
    add("causal16", (np.where(ii >= jj, 1.0, 0.0) / 16.0).astype(np.float32))
    sel = np.zeros((4, 512), np.float32)
    for h in range(4):
        sel[h, h * 128:(h + 1) * 128] = 1.0
    add("sel", sel)
    add("ones4", np.ones((4, 128), np.float32))
    add("eye4", np.eye(4, dtype=np.float32))
    rr = np.ones((4, 128), np.float32)
    rr[:, 0] = 0.0
    add("resetrow", rr)
    return np.concatenate(cols, axis=1)


CONSTS = _build_consts()
NCONST = CONSTS.shape[1]

SMALL_COLS = {}


def _fm(v):
    v = np.asarray(v, np.float32)
    lead = v.shape[:-1]
    nch = v.shape[-1] // 128
    a = v.reshape(lead + (nch, 128))
    a = np.moveaxis(a, -1, 0)
    return np.ascontiguousarray(a).reshape(128, -1)


def _build_smalls(inp):
    cols = []
    off = 0

    def add(name, arr):
        nonlocal off
        SMALL_COLS[name] = (off, arr.shape[1])
        off += arr.shape[1]
        cols.append(arr.astype(np.float32))
    add("b_ada", _fm(inp["b_ada"]))
    add("norm_mix", _fm(inp["norm_mix"]))
    add("norm_mlp", _fm(inp["norm_mlp"]))
    add("lru_conv_w", _fm(inp["lru_conv_w"]))
    add("lru_conv_b", _fm(inp["lru_conv_b"]))
    add("lru_b_r", _fm(inp["lru_b_r"]))
    add("lru_b_i", _fm(inp["lru_b_i"]))
    add("lru_lambda", _fm(inp["lru_lambda"]))
    add("m_conv_w", _fm(inp["m_conv_w"]))
    add("m_conv_b", _fm(inp["m_conv_b"]))
    add("m_norm", _fm(inp["m_norm"]))
    add("final_norm", _fm(inp["final_norm"]))
    wif = np.asarray(inp["m_w_if"], np.float32).reshape(L, 24, 128, 8).transpose(2, 0, 1, 3).reshape(128, L * 24 * 8)
    add("m_w_if", wif)
    bif = np.zeros((128, L), np.float32)
    bif[0:8, :] = np.asarray(inp["m_b_if"], np.float32).T
    add("m_b_if", bif)
    return np.ascontiguousarray(np.concatenate(cols, axis=1))


def _blockdiag(w, transpose=False):
    w = np.asarray(w, np.float32)
    Lw, nb, bs, bo = w.shape
    full = np.zeros((Lw, nb * bs, nb * bo), np.float32)
    for n in range(nb):
        blk = w[:, n]
        if transpose:
            blk = blk.transpose(0, 2, 1)
        full[:, n * bs:(n + 1) * bs, n * bo:(n + 1) * bo] = blk
    out = np.zeros((Lw, 128, 8, 128), np.float32)
    for c in range(8):
        out[:, :, c, :] = full[:, c * 128:(c + 1) * 128, c * 128:(c + 1) * 128]
    return out


class Gen:
    def __init__(self, nlayers=L, debug=None, record=None):
        self.nl = nlayers
        self.debug = debug or []
        self.wlist = record
        self.recording = record is None
        self.wspecs = []
        self.widx = 0
        self.wissued = 0
        self.res = {}
        self.ybump = 0

    def R(self, key):
        r = self.res.get(key)
        if r is None:
            r = Res()
            self.res[key] = r
        return r

    def setup(self):
        nc = bass.Bass("TRN2", target_bir_lowering=False)
        self.nc = nc
        self.S = Sched(nc, record_only=self.recording)
        dt = nc.dram_tensor
        self.d = {}
        self.d["x"] = dt("x", [SEQ, D], F32, kind="ExternalInput").ap()
        self.d["cvec"] = dt("cvec", [128, 8], F32, kind="ExternalInput").ap()
        self.d["pos"] = dt("pos", [SEQ], I32, kind="ExternalInput").ap()
        self.d["consts"] = dt("consts", [128, NCONST], F32, kind="ExternalInput").ap()
        self.d["smalls"] = dt("smalls", [128, self.nsmall], F32, kind="ExternalInput").ap()
        self.d["bd"] = dt("bd", [L, 8, 128, 8, 128], F32, kind="ExternalInput").ap()
        self.d["w_ada"] = dt("w_ada", [L, D, 6 * D], F32, kind="ExternalInput").ap()
        self.d["w_in"] = dt("w_in", [L, D, D_IN], F32, kind="ExternalInput").ap()
        for n in ("w_br_ret", "w_br_lru", "w_br_mlstm", "w_out"):
            self.d[n] = dt(n, [L, D, D], F32, kind="ExternalInput").ap()
        self.d["w_ff1"] = dt("w_ff1", [L, D, 4 * D], F32, kind="ExternalInput").ap()
        self.d["w_ff2"] = dt("w_ff2", [L, 4 * D, D], F32, kind="ExternalInput").ap()
        self.d["out"] = dt("out", [SEQ, D], F32, kind="ExternalOutput").ap()
        self.d["xs"] = dt("xs_scratch", [128, NCH, SEQ], F32).ap()
        self.dbg_out = {}
        for name, shape in self.debug:
            self.dbg_out[name] = dt("dbg_" + name, list(shape), F32, kind="ExternalOutput").ap()
        a = nc.alloc_sbuf_tensor
        self.RA = a("RA", [128, 32768], BF16)
        self.RB = a("RB", [128, 32768], BF16)
        self.RM = a("RM", [128, 16384], BF16)
        self.ring = [a("ring%d" % i, [128, 8, 256], BF16) for i in range(NB_RING)]
        self.consts = a("consts_sb", [128, NCONST], F32)
        self.smalls = a("smalls_sb", [128, self.nsmall], F32)
        self.bdw = a("bdw", [128, 5, 8, 128], BF16)
        self.onesb = a("onesb", [128, 128], BF16)
        self.condb = a("condb", [128, 8], BF16)
        self.modv = a("modv", [128, 48], F32)
        self.modn = a("modn", [128, 48], F32)
        self.vecs = a("vecs", [128, 8, 8], F32)
        self.fsq0 = a("fsq0", [128, TB], F32)
        self.fsq1 = a("fsq1", [128, TB], F32)
        self.ps = nc.alloc_psum_tensor("ps", [128, 4096], F32)
        self.X = self.RA
        self.Y = self.RB

    def C(self, name):
        o, n = CONST_COLS[name]
        return self.consts[:, o:o + n]

    def SM(self, name, l=None, per=None):
        o, n = SMALL_COLS[name]
        if l is None:
            return self.smalls[:, o:o + n]
        return self.smalls[:, o + l * per:o + (l + 1) * per]

    def bank(self, i):
        return self.ps[:, i * 512:(i + 1) * 512]

    def PB(self, i):
        r = self.res.get(("ps", i))
        if r is None:
            r = Res(excl=True)
            self.res[("ps", i)] = r
        return r

    def xview(self, reg):
        return reg[:, :].bitcast(F32).rearrange("p (c t) -> p c t", c=NCH)

    def hview(self, reg, off=0):
        return reg[:, off:off + 16384].rearrange("p (c t) -> p c t", c=NCH)

    def ytemp(self, n_elems, dtype):
        sz = n_elems * (2 if dtype == F32 else 1)
        if self.ybump % 2:
            self.ybump += 1
        o = self.ybump
        self.ybump += sz
        assert self.ybump <= 32768, "Y temp overflow %d" % self.ybump
        v = self.Y[:, o:o + sz]
        if dtype == F32:
            v = v.bitcast(F32)
        return v

    def wget(self, name, l, kg, c0, ncols):
        spec = (name, l, kg, c0, ncols)
        if self.recording:
            self.wspecs.append(spec)
            return self.ring[0][:, :, 0:ncols], self.R(("ring", 0))
        i = self.widx
        assert self.wlist[i] == spec, (i, self.wlist[i], spec)
        self.widx += 1
        while self.wissued < len(self.wlist) and self.wissued <= i + NB_RING - 1:
            j = self.wissued
            n2, l2, kg2, c2, nc2 = self.wlist[j]
            slot = j % NB_RING
            src = self.d[n2][l2, kg2 * 1024:(kg2 + 1) * 1024, c2:c2 + nc2].rearrange("(k p) n -> p k n", p=128)
            self.S.dma("pool", self.ring[slot][:, :, 0:nc2], src, writes=[self.R(("ring", slot))])
            self.wissued += 1
        slot = i % NB_RING
        return self.ring[slot][:, :, 0:ncols], self.R(("ring", slot))

    def mm(self, out, lhsT, rhs, start, stop, reads, writes):
        self.S.op("pe", "matmul", reads=reads, writes=writes, out=out, lhsT=lhsT, rhs=rhs, start=start, stop=stop)

    def act(self, out, in_, func, reads, writes, scale=1.0, bias=0.0):
        kw = dict(out=out, in_=in_, func=func)
        if not (isinstance(scale, float) and scale == 1.0):
            kw["scale"] = scale
        if not (isinstance(bias, float) and bias == 0.0):
            kw["bias"] = bias
        self.S.op("act", "activation", reads=reads, writes=writes, **kw)

    def tt(self, out, in0, in1, op, reads, writes, eng="dve"):
        self.S.op(eng, "tensor_tensor", reads=reads, writes=writes, out=out, in0=in0, in1=in1, op=op)

    def ts(self, out, in0, s1, s2, op0, op1, reads, writes, eng="dve"):
        if s2 is None:
            self.S.op(eng, "tensor_scalar", reads=reads, writes=writes, out=out, in0=in0, scalar1=s1, scalar2=None, op0=op0)
        else:
            self.S.op(eng, "tensor_scalar", reads=reads, writes=writes, out=out, in0=in0, scalar1=s1, scalar2=s2, op0=op0, op1=op1)

    def stt(self, out, in0, scalar, in1, op0, op1, reads, writes, eng="dve"):
        self.S.op(eng, "scalar_tensor_tensor", reads=reads, writes=writes, out=out, in0=in0, scalar=scalar, in1=in1, op0=op0, op1=op1)

    def copy(self, out, in_, reads, writes, eng="dve"):
        if eng == "act":
            self.act(out, in_, AF.Copy, reads, writes)
        else:
            self.S.op(eng, "tensor_copy", reads=reads, writes=writes, out=out, in_=in_)

    def dump(self, name, ap, reads):
        if name in self.dbg_out:
            self.S.barrier()
            t = self.S.dma("sp", self.dbg_out[name], ap, reads=reads)
            self.S.barrier()

    def proj_fm(self, blk, rblk, coff, hsrc, hkey, banks, kchunks=NCH):
        for tb in range(NTB):
            b = banks[tb]
            for k in range(kchunks):
                self.mm(self.bank(b), blk[:, k, coff:coff + 128], hsrc[:, k, tb * TB:(tb + 1) * TB],
                        k == 0, k == kchunks - 1, [rblk, self.R((hkey, tb))], [self.PB(b)])

    def rstd_from_bank(self, b, out, n, rout):
        self.ts(out, self.bank(b), 1.0 / n, EPS, ALU.mult, ALU.add, [self.PB(b)], [rout])
        self.act(out, out, AF.Sqrt, [rout], [rout])
        self.S.op("dve", "reciprocal", reads=[rout], writes=[rout], out=out, in_=out)

    def norm(self, xv, xkey, Av, Bv, dst, dkey, tmp_pool):
        sq, rsq, rs, rrs, tmp, rtmp = tmp_pool
        for tb in range(NTB):
            sl = slice(tb * TB, (tb + 1) * TB)
            b = 4 + tb
            for c in range(NCH):
                i = (tb * NCH + c) % 2
                self.act(sq[i], xv[:, c, sl], AF.Square, [self.R((xkey, c, tb))], [rsq[i]])
                self.mm(self.bank(b), self.onesb[:, :], sq[i], c == 0, c == NCH - 1, [rsq[i], self.R("onesb")], [self.PB(b)])
        for tb in range(NTB):
            self.rstd_from_bank(4 + tb, rs[tb], float(D), rrs[tb])
        for tb in range(NTB):
            sl = slice(tb * TB, (tb + 1) * TB)
            for c in range(NCH):
                i = (tb * NCH + c) % 2
                self.tt(tmp[i], xv[:, c, sl], rs[tb], ALU.mult, [self.R((xkey, c, tb)), rrs[tb]], [rtmp[i]])
                self.act(dst[:, c, sl], tmp[i], AF.Identity, [rtmp[i], self.R("vecs")], [self.R((dkey, tb))],
                         scale=Av[:, c:c + 1], bias=Bv[:, c:c + 1])

    def norm_tmps(self, alloc):
        sq = [alloc(TB, BF16) for _ in range(2)]
        rs = [alloc(TB, F32) for _ in range(NTB)]
        tmp = [alloc(TB, F32) for _ in range(2)]
        return (sq, [Res(), Res()], rs, [Res() for _ in range(NTB)], tmp, [Res(), Res()])

    def prologue(self):
        S = self.S
        S.dma("sp", self.consts[:, :], self.d["consts"], writes=[self.R("consts")])
        S.dma("sp", self.smalls[:, :], self.d["smalls"], writes=[self.R("smalls")])
        self.ybump = 0
        cf = self.ytemp(8, F32)
        rcf = Res()
        S.dma("sp", cf, self.d["cvec"], writes=[rcf])
        self.act(self.condb[:, :], cf, AF.Silu, [rcf], [self.R("condb")])
        S.op("dve", "memset", reads=[], writes=[self.R("onesb")], ap=self.onesb[:, :], constant=1.0)
        S.barrier()
        self.ybump = 0
        xs = [self.ytemp(D, F32) for _ in range(2)]
        rxs = [Res(), Res()]
        xv = self.xview(self.X)
        ident = self.C("ident")
        for tt_ in range(16):
            i = tt_ % 2
            S.dma("sp", xs[i], self.d["x"][tt_ * 128:(tt_ + 1) * 128, :], writes=[rxs[i]])
            tb = tt_ // 4
            for half in range(2):
                b = (tt_ * 2 + half) % 8
                for cc in range(4):
                    c = half * 4 + cc
                    S.op("pe", "transpose", reads=[rxs[i], self.R("consts")], writes=[self.PB(b)],
                         out=self.bank(b)[:, cc * 128:(cc + 1) * 128], in_=xs[i][:, c * 128:(c + 1) * 128], identity=ident)
                eng = "act" if half == 0 else "dve"
                self.copy(xv[:, half * 4:(half + 1) * 4, tt_ * 128:(tt_ + 1) * 128],
                          self.bank(b).rearrange("p (c t) -> p c t", c=4), [self.PB(b)],
                          [self.R(("x", half * 4 + cc, tb)) for cc in range(4)], eng=eng)
        S.barrier()

    def rotary_tables(self):
        S = self.S
        self.cosT = self.ytemp(SEQ, BF16)
        self.sinT = self.ytemp(SEQ, BF16)
        keep = self.ybump
        posi = self.ytemp(SEQ, F32).bitcast(I32)
        rpos = Res()
        S.dma("sp", posi, self.d["pos"].partition_broadcast(128), writes=[rpos])
        ang = self.ytemp(TB, F32)
        u = self.ytemp(TB, F32)
        ki = self.ytemp(TB, F32).bitcast(I32)
        kf = self.ytemp(TB, F32)
        ru = Res()
        for tb in range(NTB):
            sl = slice(tb * TB, (tb + 1) * TB)
            self.copy(ang, posi[:, sl], [rpos, ru], [ru])
            self.ts(ang, ang, self.C("invfreq"), None, ALU.mult, None, [ru, self.R("consts")], [ru])
            for dst, shift in ((self.sinT, math.pi), (self.cosT, 1.5 * math.pi)):
                self.ts(u, ang, shift, None, ALU.add, None, [ru], [ru])
                self.ts(ki, u, 1.0 / (2 * math.pi), None, ALU.mult, None, [ru], [ru])
                self.copy(kf, ki, [ru], [ru])
                self.stt(u, kf, -2 * math.pi, u, ALU.mult, ALU.add, [ru], [ru])
                self.ts(kf, u, 0.0, 2 * math.pi, ALU.is_lt, ALU.mult, [ru], [ru])
                self.tt(u, u, kf, ALU.add, [ru], [ru])
                self.ts(u, u, -math.pi, math.pi, ALU.add, ALU.min, [ru], [ru])
                self.ts(u, u, -math.pi, None, ALU.max, None, [ru], [ru])
                self.act(dst[:, sl], u, AF.Sin, [ru], [self.R("rot")])
        S.barrier()
        self.ybump = keep

    def adaln_block(self, l, blk_i, b, dst, rdst):
        blk, rblk = self.wget("w_ada", l, 0, blk_i * 256, 256)
        for fc in range(2):
            for k in range(NCH):
                self.mm(self.bank(b)[:, fc:fc + 1], blk[:, k, fc * 128:(fc + 1) * 128], self.condb[:, k:k + 1],
                        k == 0, k == NCH - 1, [rblk, self.R("condb")], [self.PB(b)])
        self.copy(dst[:, blk_i * 2:blk_i * 2 + 2], self.bank(b)[:, 0:2], [self.PB(b)], [rdst])

    def adaln(self, l, precomputed=False):
        S = self.S
        rv = self.R("vecs")
        if not precomputed:
            for blk_i in range(24):
                self.adaln_block(l, blk_i, 7, self.modn, self.R("modn"))
        self.tt(self.modv[:, :], self.modn[:, :], self.SM("b_ada", l, 48), ALU.add, [self.R("modn"), self.R("smalls"), rv], [rv])
        self.stt(self.vecs[:, 0, :], self.modv[:, 8:16], 1.0, self.SM("norm_mix", l, 8), ALU.add, ALU.mult, [rv, self.R("smalls")], [rv])
        self.stt(self.vecs[:, 1, :], self.modv[:, 32:40], 1.0, self.SM("norm_mlp", l, 8), ALU.add, ALU.mult, [rv, self.R("smalls")], [rv])
        self.act(self.vecs[:, 4, :], self.SM("lru_lambda", l, 8), AF.Exp, [self.R("smalls"), rv], [rv], scale=-1.0)
        self.act(self.vecs[:, 4, :], self.vecs[:, 4, :], AF.Ln, [rv], [rv], bias=1.0)
        self.ts(self.vecs[:, 2, :], self.vecs[:, 4, :], -8.0, None, ALU.mult, None, [rv], [rv])
        self.ts(self.vecs[:, 3, :], self.vecs[:, 4, :], -16.0, None, ALU.mult, None, [rv], [rv])
        for kind in range(5):
            S.dma("pool", self.bdw[:, kind, :, :], self.d["bd"][l, kind], writes=[self.R("bdw")])

    def branch_merge(self, l, bidx, wname):
        bo = self.hview(self.X, 0)
        mg = self.hview(self.X, 16384)
        hT = self.hview(self.RM)
        gsb = [self.ytemp(SEQ, BF16) for _ in range(2)]
        rg = [Res(), Res()]
        tmpm = [self.ytemp(TB, BF16) for _ in range(2)]
        rtm = [Res(), Res()]
        for f in range(NCH):
            gi = f % 2
            blk, rblk = self.wget("w_in", l, 0, OFF_GATE + bidx * 1024 + f * 128, 128)
            self.proj_fm(blk, rblk, 0, hT, "h", [0, 1, 2, 3])
            for tb in range(NTB):
                self.act(gsb[gi][:, tb * TB:(tb + 1) * TB], self.bank(tb), AF.Sigmoid, [self.PB(tb)], [rg[gi]])
            wblk, rwblk = self.wget(wname, l, 0, f * 128, 128)
            self.proj_fm(wblk, rwblk, 0, bo, "bo", [4, 5, 6, 7])
            for tb in range(NTB):
                sl = slice(tb * TB, (tb + 1) * TB)
                if bidx == 0:
                    self.tt(mg[:, f, sl], self.bank(4 + tb), gsb[gi][:, sl], ALU.mult, [self.PB(4 + tb), rg[gi]], [self.R(("mg", tb))])
                else:
                    i = tb % 2
                    self.tt(tmpm[i], self.bank(4 + tb), gsb[gi][:, sl], ALU.mult, [self.PB(4 + tb), rg[gi]], [rtm[i]])
                    self.tt(mg[:, f, sl], mg[:, f, sl], tmpm[i], ALU.add, [rtm[i], self.R(("mg", tb))], [self.R(("mg", tb))])

    def lru_branch(self, l):
        S = self.S
        self.ybump = 0
        bo = self.hview(self.X, 0)
        hT = self.hview(self.RM)
        xl = self.ytemp(SEQ, F32)
        a = self.ytemp(SEQ, F32)
        u = self.ytemp(SEQ, F32)
        rsb = self.ytemp(SEQ, F32)
        isb = self.ytemp(SEQ, F32)
        t1 = self.ytemp(SEQ, F32)
        xlb = self.ytemp(SEQ, BF16)
        rxl, ra, ru, rxlb, rrs, ris, rt1 = Res(), Res(), Res(), Res(), Res(), Res(), Res()
        rsm = self.R("smalls")
        rv = self.R("vecs")
        A_lo = self.ps[:, 0:SEQ]
        A_hi = self.ps[:, SEQ:2 * SEQ]
        lo = [self.PB(i) for i in range(4)]
        hi = [self.PB(4 + i) for i in range(4)]
        cw = self.SM("lru_conv_w", l, 32)
        cb = self.SM("lru_conv_b", l, 8)
        for c in range(NCH):
            blk, rblk = self.wget("w_in", l, 0, OFF_LX + c * 128, 128)
            self.proj_fm(blk, rblk, 0, hT, "h", [0, 1, 2, 3])
            self.ts(xl, A_lo, cw[:, 3 * 8 + c:3 * 8 + c + 1], cb[:, c:c + 1], ALU.mult, ALU.add, lo + [rsm], [rxl])
            for s_ in range(1, 4):
                self.stt(xl[:, s_:SEQ], A_lo[:, 0:SEQ - s_], cw[:, (3 - s_) * 8 + c:(3 - s_) * 8 + c + 1], xl[:, s_:SEQ],
                         ALU.mult, ALU.add, lo + [rsm, rxl], [rxl])
            self.copy(xlb, xl, [rxl], [rxlb], eng="act")
            for tb in range(NTB):
                sl = slice(tb * TB, (tb + 1) * TB)
                self.mm(self.bank(4 + tb), self.bdw[:, 0, c, :], xlb[:, sl], True, True, [rxlb, self.R("bdw")], [self.PB(4 + tb)])
            for tb in range(NTB):
                sl = slice(tb * TB, (tb + 1) * TB)
                self.mm(self.bank(tb), self.bdw[:, 1, c, :], xlb[:, sl], True, True, [rxlb, self.R("bdw")], [self.PB(tb)])
            self.act(rsb, A_hi, AF.Sigmoid, hi + [rsm], [rrs], bias=self.SM("lru_b_r", l, 8)[:, c:c + 1])
            self.act(isb, A_lo, AF.Sigmoid, lo + [rsm], [ris], bias=self.SM("lru_b_i", l, 8)[:, c:c + 1])
            blk, rblk = self.wget("w_in", l, 0, OFF_LY + c * 128, 128)
            self.proj_fm(blk, rblk, 0, hT, "h", [4, 5, 6, 7])
            self.act(a, rsb, AF.Exp, [rrs, rv], [ra], scale=self.vecs[:, 2, c:c + 1])
            self.act(t1, rsb, AF.Exp, [rrs, rv], [rt1], scale=self.vecs[:, 3, c:c + 1])
            self.ts(t1, t1, -1.0, 1.0, ALU.mult, ALU.add, [rt1], [rt1])
            self.act(t1, t1, AF.Sqrt, [rt1], [rt1])
            self.tt(u, isb, xl, ALU.mult, [ris, rxl], [ru])
            self.tt(u, u, t1, ALU.mult, [ru, rt1], [ru])
            S.op("dve", "tensor_tensor_scan", reads=[ra, ru, rxl], writes=[rxl], out=xl, data0=a, data1=u, initial=0.0,
                 op0=ALU.mult, op1=ALU.add)
            self.act(t1, A_hi, AF.Square, hi + [rt1], [rt1])
            self.ts(t1, t1, 0.0713548163, 1.5957691216, ALU.mult, ALU.add, [rt1], [rt1])
            self.tt(t1, t1, A_hi, ALU.mult, [rt1] + hi, [rt1])
            self.act(t1, t1, AF.Sigmoid, [rt1], [rt1])
            self.tt(t1, t1, A_hi, ALU.mult, [rt1] + hi, [rt1])
            self.tt(bo[:, c, :], t1, xl, ALU.mult, [rt1, rxl], [self.R(("bo", tb)) for tb in range(NTB)])

    def ret_branch(self, l):
        S = self.S
        bo = self.hview(self.X, 0)
        hT = self.hview(self.RM)
        rc = self.R("consts")
        self.ybump = 0
        self.rotary_tables()
        qT = self.ytemp(SEQ, BF16)
        qsT = self.ytemp(SEQ, BF16)
        kT = self.ytemp(SEQ, BF16)
        kd = self.ytemp(16 * 128, BF16).rearrange("p (c d) -> p c d", c=16)
        vt = self.ytemp(16 * 256, BF16).rearrange("p (c e) -> p c e", c=16)
        srg = self.ytemp(2 * SEQ, BF16).rearrange("p (c t) -> p c t", c=2)
        pblk = self.ytemp(8 * 128, BF16).rearrange("p (k n) -> p k n", k=8)
        t1 = [self.ytemp(TB, F32) for _ in range(2)]
        t2 = [self.ytemp(TB, F32) for _ in range(2)]
        Sst = self.ytemp(256, F32)
        Sbf = self.ytemp(256, BF16)
        sT = [self.ytemp(128, BF16) for _ in range(2)]
        sq = self.ytemp(2 * TB, BF16).rearrange("p (c t) -> p c t", c=2)
        rs = self.ytemp(TB, F32)
        tmp = self.ytemp(TB, F32)
        rq, rqs, rk, rkd, rvt, rsrg, rpb = Res(), Res(), Res(), Res(), Res(), Res(), Res()
        rt1, rt2 = [Res(), Res()], [Res(), Res()]
        rS, rSb, rsT, rsq, rrs, rtmp = Res(), Res(), [Res(), Res()], Res(), Res(), Res()
        ident = self.C("ident")
        for h in range(4):
            for which in range(2):
                off = (OFF_RQ if which == 0 else OFF_RK) + h * 128
                blk, rblk = self.wget("w_in", l, 0, off, 128)
                self.act(pblk[:, :, 0:64], blk[:, :, 64:128], AF.Copy, [rblk], [rpb], scale=-1.0)
                self.copy(pblk[:, :, 64:128], blk[:, :, 0:64], [rblk], [rpb], eng="dve")
                self.proj_fm(blk, rblk, 0, hT, "h", [0, 1, 2, 3])
                self.proj_fm(pblk, rpb, 0, hT, "h", [4, 5, 6, 7])
                for tb in range(NTB):
                    sl = slice(tb * TB, (tb + 1) * TB)
                    i = tb % 2
                    self.tt(t1[i], self.bank(tb), self.cosT[:, sl], ALU.mult, [self.PB(tb), self.R("rot")], [rt1[i]])
                    self.tt(t2[i], self.bank(4 + tb), self.sinT[:, sl], ALU.mult, [self.PB(4 + tb), self.R("rot")], [rt2[i]])
                    self.tt(t1[i], t1[i], t2[i], ALU.add, [rt1[i], rt2[i]], [rt1[i]])
                    if which == 0:
                        self.copy(qT[:, sl], t1[i], [rt1[i]], [rq], eng="act")
                        dq = self.C("dq%d" % h).unsqueeze(1).broadcast_to([128, 4, 128])
                        self.tt(qsT[:, sl].rearrange("p (a b) -> p a b", a=4), t1[i].rearrange("p (a b) -> p a b", a=4), dq,
                                ALU.mult, [rt1[i], rc], [rqs])
                    else:
                        self.copy(kT[:, sl], t1[i], [rt1[i]], [rk], eng="act")
                        for cc in range(4):
                            S.op("pe", "transpose", reads=[rt1[i], rc], writes=[self.PB(tb)],
                                 out=self.bank(tb)[:, cc * 128:(cc + 1) * 128], in_=t1[i][:, cc * 128:(cc + 1) * 128], identity=ident)
                        self.ts(kd[:, tb * 4:(tb + 1) * 4, :], self.bank(tb).rearrange("p (c d) -> p c d", c=4),
                                self.C("kdscale")[:, h:h + 1], None, ALU.mult, None, [self.PB(tb), rc], [rkd])
            blk, rblk = self.wget("w_in", l, 0, OFF_RV + h * 256, 256)
            for tt_ in range(16):
                b = tt_ // 2 % 8
                o = (tt_ % 2) * 256
                for k in range(NCH):
                    self.mm(self.bank(b)[:, o:o + 256], hT[:, k, tt_ * 128:(tt_ + 1) * 128], blk[:, k, :], k == 0, k == NCH - 1,
                            [rblk, self.R(("h", tt_ // 4))], [self.PB(b)])
                if tt_ % 2 == 1:
                    self.copy(vt[:, tt_ - 1:tt_ + 1, :], self.bank(b).rearrange("p (c e) -> p c e", c=2), [self.PB(b)], [rvt],
                              eng="act" if (tt_ // 2) % 2 else "dve")
            blk, rblk = self.wget("w_in", l, 0, OFF_RG + h * 256, 256)
            for ec in range(2):
                banks = [0, 1, 2, 3] if ec == 0 else [4, 5, 6, 7]
                self.proj_fm(blk, rblk, ec * 128, hT, "h", banks)
                for tb in range(NTB):
                    self.act(srg[:, ec, tb * TB:(tb + 1) * TB], self.bank(banks[tb]), AF.Silu, [self.PB(banks[tb])], [rsrg])
            g128 = GAMMA[h] ** 128.0
            mask = self.C("retmask%d" % h)
            for c in range(16):
                tb = c // 4
                cs = slice(c * 128, (c + 1) * 128)
                si = c % 2
                bs = si
                ob = [2 + 2 * (tb % 2), 3 + 2 * (tb % 2)]
                oc = slice((c % 4) * 128, (c % 4 + 1) * 128)
                self.mm(self.bank(bs)[:, 0:128], kT[:, cs], qT[:, cs], True, True, [rk, rq], [self.PB(bs)])
                if c < 15:
                    self.mm(self.bank(6)[:, 0:256], kd[:, c, :], vt[:, c, :], True, True, [rkd, rvt], [self.PB(6)])
                self.tt(sT[si], self.bank(bs)[:, 0:128], mask, ALU.mult, [self.PB(bs), rc], [rsT[si]])
                if c < 15:
                    if c == 0:
                        self.copy(Sst, self.bank(6)[:, 0:256], [self.PB(6)], [rS])
                    else:
                        self.stt(Sst, Sst, g128, self.bank(6)[:, 0:256], ALU.mult, ALU.add, [rS, self.PB(6)], [rS])
                for ec in range(2):
                    es = slice(ec * 128, (ec + 1) * 128)
                    self.mm(self.bank(ob[ec])[:, oc], vt[:, c, es], sT[si], True, c == 0, [rvt, rsT[si]], [self.PB(ob[ec])])
                    if c > 0:
                        self.mm(self.bank(ob[ec])[:, oc], Sbf[:, es], qsT[:, cs], False, True, [rSb, rqs], [self.PB(ob[ec])])
                if c < 15:
                    self.copy(Sbf, Sst, [rS], [rSb], eng="act")
                if c % 4 == 3:
                    sl = slice(tb * TB, (tb + 1) * TB)
                    for ec in range(2):
                        self.act(sq[:, ec, :], self.bank(ob[ec]), AF.Square, [self.PB(ob[ec])], [rsq])
                    for ec in range(2):
                        self.mm(self.bank(7), self.onesb[:, :], sq[:, ec, :], ec == 0, ec == 1, [rsq, self.R("onesb")], [self.PB(7)])
                    self.rstd_from_bank(7, rs, 256.0, rrs)
                    for ec in range(2):
                        self.tt(tmp, self.bank(ob[ec]), rs, ALU.mult, [self.PB(ob[ec]), rrs], [rtmp])
                        self.tt(bo[:, 2 * h + ec, sl], tmp, srg[:, ec, sl], ALU.mult, [rtmp, rsrg], [self.R(("bo", tb))])

    def conv_tb(self, A_all, pbs, cw, cb, ch, tb, dst, rdst, rsm):
        t0 = tb * TB
        pbs = [self.PB(tb)] + ([self.PB(tb - 1)] if tb > 0 else [])
        self.ts(dst, A_all[:, t0:t0 + TB], cw[:, 3 * 8 + ch:3 * 8 + ch + 1], cb[:, ch:ch + 1], ALU.mult, ALU.add, [self.PB(tb), rsm], [rdst])
        for s in range(1, 4):
            lo = s if tb == 0 else 0
            self.stt(dst[:, lo:TB], A_all[:, t0 + lo - s:t0 + TB - s], cw[:, (3 - s) * 8 + ch:(3 - s) * 8 + ch + 1], dst[:, lo:TB],
                     ALU.mult, ALU.add, pbs + [rsm, rdst], [rdst])

    def mlstm_branch(self, l):
        S = self.S
        bo = self.hview(self.X, 0)
        hT = self.hview(self.RM)
        rc = self.R("consts")
        rsm = self.R("smalls")
        rbd = self.R("bdw")
        A_all = self.ps[:, 0:SEQ]
        cw = self.SM("m_conv_w", l, 32)
        cb = self.SM("m_conv_b", l, 8)
        ident = self.C("ident")
        self.ybump = 0
        bT = self.ytemp(SEQ, F32)
        decb = self.ytemp(64, F32)
        wkt = self.ytemp(64, F32)
        rb, rdecb, rwkt = Res(), Res(), Res()
        base = self.ybump
        bdT = self.ytemp(3 * NCH * 128, F32).rearrange("p (k c n) -> p k c n", k=3, c=NCH)
        rbdT = Res()
        for kind in range(3):
            S.dma("sp", bdT[:, kind, :, :], self.d["bd"][l, 5 + kind], writes=[rbdT])
        wab = self.ytemp(2 * NCH * 128, BF16).rearrange("p (a c g) -> p a c g", a=2, c=NCH)
        rwab = Res()
        S.op("dve", "memset", reads=[], writes=[rwab], ap=wab, constant=0.0)
        S.op("dve", "memset", reads=[], writes=[rb], ap=bT, constant=0.0)
        wif = self.SM("m_w_if", l, 192).rearrange("p (c g) -> p c g", c=24)
        for c in range(NCH):
            self.mm(self.bank(7)[:, c * 16:c * 16 + 8], bdT[:, 0, c, :], wif[:, c, :], True, False, [rbdT, rsm], [self.PB(7)])
            self.mm(self.bank(7)[:, c * 16:c * 16 + 8], bdT[:, 1, c, :], wif[:, 8 + c, :], False, True, [rbdT, rsm], [self.PB(7)])
            self.mm(self.bank(7)[:, c * 16 + 8:c * 16 + 16], bdT[:, 2, c, :], wif[:, 16 + c, :], True, True, [rbdT, rsm], [self.PB(7)])
        b7 = self.bank(7)[:, 0:128].rearrange("p (c a g) -> p a c g", c=NCH, a=2)
        self.copy(wab[:, :, :, 0:8], b7, [self.PB(7), rwab], [rwab])
        xct = [self.ytemp(TB, F32) for _ in range(2)]
        xcb = [self.ytemp(TB, BF16) for _ in range(2)]
        mxb = [self.ytemp(TB, BF16) for _ in range(2)]
        rxct, rxcb, rmxb = [Res(), Res()], [Res(), Res()], [Res(), Res()]
        for c in range(NCH):
            blk, rblk = self.wget("w_in", l, 0, OFF_MX + c * 128, 128)
            self.proj_fm(blk, rblk, 0, hT, "h", [0, 1, 2, 3])
            pbs = [self.PB(i) for i in range(4)]
            for tb in range(NTB):
                i = tb % 2
                self.copy(mxb[i], self.bank(tb), [self.PB(tb)], [rmxb[i]], eng="act")
                self.conv_tb(A_all, pbs, cw, cb, c, tb, xct[i], rxct[i], rsm)
                self.act(xcb[i], xct[i], AF.Silu, [rxct[i]], [rxcb[i]])
                self.mm(self.bank(4 + tb), wab[:, 0, c, :], xcb[i], c == 0, False, [rwab, rxcb[i]], [self.PB(4 + tb)])
                self.mm(self.bank(4 + tb), wab[:, 1, c, :], mxb[i], False, c == NCH - 1, [rwab, rmxb[i]], [self.PB(4 + tb)])
        S.barrier()
        self.ybump = base
        ifT = self.ytemp(SEQ, F32)
        fT = self.ytemp(SEQ, F32)
        gT = self.ytemp(SEQ, F32)
        sm = self.ytemp(256, F32)
        rif, rf, rgt, rsmr = Res(), Res(), Res(), Res()
        S.op("dve", "memset", reads=[], writes=[rgt], ap=gT, constant=0.0)
        S.op("dve", "memset", reads=[], writes=[rsmr], ap=sm, constant=0.0)
        bif = self.SM("m_b_if", l, 1)
        for tb in range(NTB):
            sl = slice(tb * TB, (tb + 1) * TB)
            self.act(ifT[0:8, sl], self.bank(4 + tb)[0:8, :], AF.Identity, [self.PB(4 + tb), rsm], [rif], bias=bif[0:8, :])
        self.dump("ifpre", ifT[0:8, :], [rif])
        S.dma("sp", fT[0:4, :], ifT[4:8, :], reads=[rif], writes=[rf])
        F4, B4, G4, I4 = fT[0:4, :], bT[0:4, :], gT[0:4, :], ifT[0:4, :]
        self.stt(B4, F4, -1.0, F4, ALU.mult, ALU.max, [rf], [rb])
        self.act(B4, B4, AF.Exp, [rb], [rb], scale=-1.0)
        self.act(B4, B4, AF.Ln, [rb], [rb], bias=1.0)
        self.ts(G4, F4, 0.0, None, ALU.min, None, [rf], [rgt])
        self.tt(F4, G4, B4, ALU.subtract, [rgt, rb], [rf])
        rr = self.C("resetrow")[0:4, :].unsqueeze(1).broadcast_to([4, 16, 128])
        self.copy(G4.rearrange("p (c t) -> p c t", c=16), rr, [rc, rgt], [rgt])
        S.op("dve", "tensor_tensor_scan", reads=[rgt, rf, rb], writes=[rb], out=B4, data0=G4, data1=F4, initial=0.0,
             op0=ALU.mult, op1=ALU.add)
        self.tt(G4, I4, B4, ALU.subtract, [rif, rb, rgt], [rgt])
        Gc = sm[0:4, 0:16]
        bl = sm[0:4, 16:32]
        ms1 = sm[0:4, 32:48]
        ms0 = sm[0:4, 48:64]
        Mc = sm[0:4, 64:80]
        dec = sm[0:4, 80:96]
        S.op("dve", "tensor_reduce", reads=[rgt], writes=[rsmr], out=Gc, in_=G4.rearrange("p (c t) -> p c t", c=16),
             axis=AX.X, op=ALU.max)
        self.copy(bl, B4.rearrange("p (c t) -> p c t", c=16)[:, :, 127], [rb, rsmr], [rsmr])
        S.op("dve", "tensor_tensor_scan", reads=[rsmr], writes=[rsmr], out=ms1, data0=Gc, data1=bl, initial=0.0,
             op0=ALU.max, op1=ALU.add)
        S.op("dve", "memset", reads=[rsmr], writes=[rsmr], ap=ms0[:, 0:1], constant=0.0)
        self.copy(ms0[:, 1:16], ms1[:, 0:15], [rsmr], [rsmr])
        self.tt(Mc, ms0, Gc, ALU.max, [rsmr], [rsmr])
        self.tt(dec, ms0, Mc, ALU.subtract, [rsmr], [rsmr])
        self.act(dec, dec, AF.Exp, [rsmr], [rsmr])
        Mb = Mc.unsqueeze(2).broadcast_to([4, 16, 128])
        self.tt(G4.rearrange("p (c t) -> p c t", c=16), G4.rearrange("p (c t) -> p c t", c=16), Mb, ALU.subtract,
                [rgt, rsmr], [rgt])
        self.act(G4, G4, AF.Exp, [rgt], [rgt])
        self.tt(B4.rearrange("p (c t) -> p c t", c=16), B4.rearrange("p (c t) -> p c t", c=16), Mb, ALU.add,
                [rb, rsmr], [rb])
        dexp = sm[0:4, 96:160].rearrange("p (h c) -> p h c", h=4)
        self.tt(dexp, dec.unsqueeze(1).broadcast_to([4, 4, 16]), self.C("eye4")[0:4, :].unsqueeze(2).broadcast_to([4, 4, 16]),
                ALU.mult, [rsmr, rc], [rsmr])
        self.mm(self.bank(4)[:, 0:64], self.C("ones4"), sm[:, 96:160], True, True, [rsmr, rc], [self.PB(4)])
        self.copy(decb, self.bank(4)[:, 0:64], [self.PB(4)], [rdecb])
        for c in range(16):
            S.op("pe", "transpose", reads=[rgt, rc], writes=[self.PB(c // 4)], out=self.bank(c // 4)[:, (c % 4) * 128:(c % 4 + 1) * 128],
                 in_=gT[:, c * 128:(c + 1) * 128], identity=ident)
        for cb4 in range(4):
            self.copy(wkt[:, cb4 * 16:(cb4 + 1) * 16].rearrange("p (c h) -> p c h", c=4),
                      self.bank(cb4).rearrange("p (c t) -> p c t", c=4)[:, :, 0:4], [self.PB(cb4)], [rwkt])
        S.barrier()
        self.ybump = base
        mxh = self.ytemp(2 * SEQ, BF16).rearrange("p (c t) -> p c t", c=2)
        xch = self.ytemp(2 * SEQ, BF16).rearrange("p (c t) -> p c t", c=2)
        smo = self.ytemp(2 * SEQ, BF16).rearrange("p (c t) -> p c t", c=2)
        xct = [self.ytemp(TB, F32) for _ in range(2)]
        qTb = self.ytemp(2 * TB, BF16).rearrange("p (c t) -> p c t", c=2)
        kTb = self.ytemp(2 * TB, BF16).rearrange("p (c t) -> p c t", c=2)
        kw = self.ytemp(4 * 256, BF16).rearrange("p (c d) -> p c d", c=4)
        vx = self.ytemp(4 * 384, BF16).rearrange("p (c e) -> p c e", c=4)
        Cst = self.ytemp(2 * 384, F32).rearrange("p (c e) -> p c e", c=2)
        Cbf = self.ytemp(2 * 384, BF16).rearrange("p (c e) -> p c e", c=2)
        sT = [self.ytemp(128, BF16) for _ in range(2)]
        thb = self.ytemp(TB, F32)
        rcp = self.ytemp(TB, F32)
        hm = self.ytemp(2 * TB, F32).rearrange("p (c t) -> p c t", c=2)
        sq = self.ytemp(2 * TB, BF16).rearrange("p (c t) -> p c t", c=2)
        rs = self.ytemp(TB, F32)
        rmxh, rxch, rqT, rkT, rkw, rvx, rsmo, rC, rCb = Res(), Res(), Res(), Res(), Res(), Res(), Res(), Res(), Res()
        rsT, rthb, rrcp, rhm, rsq, rrs = [Res(), Res()], Res(), Res(), Res(), Res(), Res()
        rxct = [Res(), Res()]
        S.op("dve", "memset", reads=[], writes=[rvx], ap=vx[:, :, 256:384], constant=1.0)
        mnorm = self.SM("m_norm", l, 8)
        for h in range(4):
            for dc in range(2):
                ch = 2 * h + dc
                blk, rblk = self.wget("w_in", l, 0, OFF_MX + ch * 128, 128)
                self.proj_fm(blk, rblk, 0, hT, "h", [0, 1, 2, 3])
                pbs = [self.PB(i) for i in range(4)]
                for tb in range(NTB):
                    i = tb % 2
                    sl = slice(tb * TB, (tb + 1) * TB)
                    self.copy(mxh[:, dc, sl], self.bank(tb), [self.PB(tb)], [rmxh], eng="act")
                    self.conv_tb(A_all, pbs, cw, cb, ch, tb, xct[i], rxct[i], rsm)
                    self.act(xch[:, dc, sl], xct[i], AF.Silu, [rxct[i]], [rxch])
            blk, rblk = self.wget("w_in", l, 0, OFF_MO + h * 256, 256)
            for ec in range(2):
                banks = [0, 1, 2, 3] if ec == 0 else [4, 5, 6, 7]
                self.proj_fm(blk, rblk, ec * 128, hT, "h", banks)
                for tb in range(NTB):
                    self.act(smo[:, ec, tb * TB:(tb + 1) * TB], self.bank(banks[tb]), AF.Sigmoid, [self.PB(banks[tb])], [rsmo])
            for tb in range(NTB):
                t0 = tb * TB
                sl = slice(t0, t0 + TB)
                for dc in range(2):
                    ch = 2 * h + dc
                    self.mm(self.bank(0), self.bdw[:, 2, ch, :], xch[:, dc, sl], True, True, [rbd, rxch], [self.PB(0)])
                    self.copy(qTb[:, dc, :], self.bank(0), [self.PB(0)], [rqT], eng="act")
                    self.mm(self.bank(1), self.bdw[:, 3, ch, :], xch[:, dc, sl], True, True, [rbd, rxch], [self.PB(1)])
                    self.copy(kTb[:, dc, :], self.bank(1), [self.PB(1)], [rkT], eng="act")
                for ci in range(4):
                    c = tb * 4 + ci
                    ts_ = slice(t0 + ci * 128, t0 + (ci + 1) * 128)
                    for dc in range(2):
                        ch = 2 * h + dc
                        self.mm(self.bank(2)[:, dc * 128:(dc + 1) * 128], xch[:, dc, ts_], self.bdw[:, 3, ch, :], True, True,
                                [rbd, rxch], [self.PB(2)])
                        self.mm(self.bank(2)[:, 256 + dc * 128:256 + (dc + 1) * 128], mxh[:, dc, ts_],
                                self.bdw[:, 4, ch, :], True, True, [rbd, rmxh], [self.PB(2)])
                    self.ts(kw[:, ci, :], self.bank(2)[:, 0:256], wkt[:, c * 4 + h:c * 4 + h + 1], 1.0 / 16.0, ALU.mult, ALU.mult,
                            [self.PB(2), rwkt], [rkw])
                    self.copy(vx[:, ci, 0:256], self.bank(2)[:, 256:512], [self.PB(2)], [rvx], eng="act")
                ob = [3, 4, 5]
                for ci in range(4):
                    c = tb * 4 + ci
                    cs = slice(ci * 128, (ci + 1) * 128)
                    si = c % 2
                    bs = si
                    idx = h * 16 + c
                    for dc in range(2):
                        self.mm(self.bank(bs)[:, 0:128], kTb[:, dc, cs], qTb[:, dc, cs], dc == 0, dc == 1, [rkT, rqT], [self.PB(bs)])
                    if c < 15:
                        for dc in range(2):
                            self.mm(self.bank(6 + dc)[:, 0:384], kw[:, ci, dc * 128:(dc + 1) * 128], vx[:, ci, :], True, True,
                                    [rkw, rvx], [self.PB(6 + dc)])
                    if c > 0:
                        for dc in range(2):
                            self.act(Cbf[:, dc, :], Cst[:, dc, :], AF.Identity, [rC, rdecb], [rCb], scale=decb[:, idx:idx + 1])
                    self.stt(sT[si], self.bank(bs)[:, 0:128], wkt[:, c * 4 + h:c * 4 + h + 1], self.C("causal16"), ALU.mult, ALU.mult,
                             [self.PB(bs), rwkt, rc], [rsT[si]])
                    if c < 15:
                        for dc in range(2):
                            if c == 0:
                                self.copy(Cst[:, dc, :], self.bank(6 + dc)[:, 0:384], [self.PB(6 + dc)], [rC])
                            else:
                                self.stt(Cst[:, dc, :], Cst[:, dc, :], decb[:, idx:idx + 1], self.bank(6 + dc)[:, 0:384],
                                         ALU.mult, ALU.add, [rC, rdecb, self.PB(6 + dc)], [rC])
                    for ec in range(3):
                        es = slice(ec * 128, (ec + 1) * 128)
                        self.mm(self.bank(ob[ec])[:, cs], vx[:, ci, es], sT[si], True, c == 0, [rvx, rsT[si]], [self.PB(ob[ec])])
                        if c > 0:
                            for dc in range(2):
                                self.mm(self.bank(ob[ec])[:, cs], Cbf[:, dc, es], qTb[:, dc, cs], False, dc == 1, [rCb, rqT],
                                        [self.PB(ob[ec])])
                self.mm(self.bank(0), self.C("sel")[:, h * 128:(h + 1) * 128], bT[:, sl], True, True, [rb, rc], [self.PB(0)])
                self.act(thb, self.bank(0), AF.Exp, [self.PB(0)], [rthb], scale=-1.0)
                self.tt(rcp, self.bank(5), thb, ALU.max, [self.PB(5), rthb], [rrcp])
                self.stt(rcp, self.bank(5), -1.0, rcp, ALU.mult, ALU.max, [self.PB(5), rrcp], [rrcp])
                S.op("dve", "reciprocal", reads=[rrcp], writes=[rrcp], out=rcp, in_=rcp)
                for ec in range(2):
                    self.tt(hm[:, ec, :], self.bank(ob[ec]), rcp, ALU.mult, [self.PB(ob[ec]), rrcp], [rhm])
                    self.tt(hm[:, ec, :], hm[:, ec, :], smo[:, ec, sl], ALU.mult, [rhm, rsmo], [rhm])
                    self.act(sq[:, ec, :], hm[:, ec, :], AF.Square, [rhm], [rsq])
                for ec in range(2):
                    self.mm(self.bank(1), self.onesb[:, :], sq[:, ec, :], ec == 0, ec == 1, [rsq, self.R("onesb")], [self.PB(1)])
                self.rstd_from_bank(1, rs, 256.0, rrs)
                for ec in range(2):
                    ch = 2 * h + ec
                    self.stt(bo[:, ch, sl], hm[:, ec, :], mnorm[:, ch:ch + 1], rs, ALU.mult, ALU.mult, [rhm, rrs, rsm],
                             [self.R(("bo", tb))])

    def layer(self, l):
        S = self.S
        X, Y = self.X, self.Y
        xv = self.xview(X)
        hT = self.hview(self.RM)
        self.adaln(l, precomputed=(l > 0))
        self.ybump = 0
        tmps = self.norm_tmps(self.ytemp)
        self.norm(xv, "x", self.vecs[:, 0, :], self.modv[:, 0:8], hT, "h", tmps)
        toks = []
        for c in range(NCH):
            toks.append(S.dma("sp", self.d["xs"][:, c, :], xv[:, c, :], reads=[self.R(("x", c, tb)) for tb in range(NTB)],
                              writes=[self.R(("xsd", c))]))
        S.barrier()
        if self.stage == "h":
            return ("bf", hT)
        for bidx, (fn, wname) in enumerate(((self.ret_branch, "w_br_ret"), (self.lru_branch, "w_br_lru"), (self.mlstm_branch, "w_br_mlstm"))):
            if self.stage in ("bo0", "bo1", "bo2") and self.stage != "bo%d" % bidx:
                continue
            fn(l)
            S.barrier()
            self.ybump = 0
            if self.stage == "bo%d" % bidx:
                return ("bf", self.hview(X, 0))
            self.branch_merge(l, bidx, wname)
            S.barrier()
        if self.stage == "mg":
            return ("bf", self.hview(X, 16384))
        yv = self.xview(Y)
        for c in range(NCH):
            S.dma("sp", yv[:, c, :], self.d["xs"][:, c, :], reads=[self.R(("xsd", c))],
                  writes=[self.R(("xn", c, tb)) for tb in range(NTB)])
        mg = self.hview(X, 16384)
        for f in range(NCH):
            if f % 2 == 0:
                blk, rblk = self.wget("w_out", l, 0, f * 128, 256)
            banks = [0, 1, 2, 3] if f % 2 == 0 else [4, 5, 6, 7]
            self.proj_fm(blk, rblk, (f % 2) * 128, mg, "mg", banks)
            for tb in range(NTB):
                sl = slice(tb * TB, (tb + 1) * TB)
                self.stt(yv[:, f, sl], self.bank(banks[tb]), self.modv[:, 16 + f:17 + f], yv[:, f, sl], ALU.mult, ALU.add,
                         [self.PB(banks[tb]), self.R("vecs"), self.R(("xn", f, tb))], [self.R(("xn", f, tb))])
        S.barrier()
        if self.stage == "xmix":
            return ("f32", yv)
        save_Y = self.Y
        self.Y = X
        self.ybump = 0
        tmps = self.norm_tmps(self.ytemp)
        self.Y = save_Y
        self.norm(yv, "xn", self.vecs[:, 1, :], self.modv[:, 24:32], hT, "h2", tmps)
        S.barrier()
        hid = X[:, :].rearrange("p (j t) -> p j t", j=32)
        fsq = [self.fsq0[:, :], self.fsq1[:, :]]
        rfsq = [self.R("fsq0"), self.R("fsq1")]
        for half in range(2):
            for jb in range(16):
                blk, rblk = self.wget("w_ff1", l, 0, jb * 256, 256)
                for jc in range(2):
                    j = jb * 2 + jc
                    for t2 in range(2):
                        tb = half * 2 + t2
                        b = (j * 2 + t2) % 7
                        for k in range(NCH):
                            self.mm(self.bank(b), blk[:, k, jc * 128:(jc + 1) * 128], hT[:, k, tb * TB:(tb + 1) * TB], k == 0, k == NCH - 1,
                                    [rblk, self.R(("h2", tb))], [self.PB(b)])
                        i2 = (j * 2 + t2) % 2
                        self.act(fsq[i2], self.bank(b), AF.Square, [self.PB(b)], [rfsq[i2]])
                        self.stt(hid[:, j, t2 * TB:(t2 + 1) * TB], self.bank(b), 0.0, fsq[i2], ALU.is_gt, ALU.mult,
                                 [self.PB(b), rfsq[i2]], [self.R(("hid", j // 8, t2))])
                if l + 1 < self.nl and jb < 12:
                    self.adaln_block(l + 1, half * 12 + jb, 7, self.modn, self.R("modn"))
            for cb_ in range(4):
                banks = [0, 1, 2, 3] if cb_ % 2 == 0 else [4, 5, 6, 7]
                for kg in range(4):
                    blk, rblk = self.wget("w_ff2", l, kg, cb_ * 256, 256)
                    for fl in range(2):
                        for t2 in range(2):
                            b = banks[fl * 2 + t2]
                            for k in range(NCH):
                                self.mm(self.bank(b), blk[:, k, fl * 128:(fl + 1) * 128], hid[:, kg * 8 + k, t2 * TB:(t2 + 1) * TB],
                                        kg == 0 and k == 0, kg == 3 and k == NCH - 1, [rblk, self.R(("hid", kg, t2))], [self.PB(b)])
                for fl in range(2):
                    f = cb_ * 2 + fl
                    for t2 in range(2):
                        tb = half * 2 + t2
                        b = banks[fl * 2 + t2]
                        sl = slice(tb * TB, (tb + 1) * TB)
                        self.stt(yv[:, f, sl], self.bank(b), self.modv[:, 40 + f:41 + f], yv[:, f, sl], ALU.mult, ALU.add,
                                 [self.PB(b), self.R("vecs"), self.R(("xn", f, tb))], [self.R(("xn", f, tb))])
            S.barrier()
        self.X, self.Y = Y, X
        for c in range(NCH):
            for tb in range(NTB):
                self.res[("x", c, tb)] = self.res.pop(("xn", c, tb))
        if self.stage == "xffn":
            return ("f32", self.xview(self.X))
        return None

    def epilogue(self):
        S = self.S
        xv = self.xview(self.X)
        self.ybump = 0
        sq = [self.ytemp(TB, BF16) for _ in range(2)]
        rsq = [Res(), Res()]
        rs = self.ytemp(TB, F32)
        rrs = Res()
        yn = self.ytemp(NCH * TB, F32).rearrange("p (c t) -> p c t", c=NCH)
        ryn = Res()
        st = [self.ytemp(D, F32) for _ in range(2)]
        rst = [Res(), Res()]
        fn = self.SM("final_norm")
        ident = self.C("ident")
        toks = []
        for tb in range(NTB):
            sl = slice(tb * TB, (tb + 1) * TB)
            for c in range(NCH):
                i = c % 2
                self.act(sq[i], xv[:, c, sl], AF.Square, [self.R(("x", c, tb))], [rsq[i]])
                self.mm(self.bank(7), self.onesb[:, :], sq[i], c == 0, c == NCH - 1, [rsq[i], self.R("onesb")], [self.PB(7)])
            self.rstd_from_bank(7, rs, float(D), rrs)
            for c in range(NCH):
                self.stt(yn[:, c, :], xv[:, c, sl], fn[:, c:c + 1], rs, ALU.mult, ALU.mult, [self.R(("x", c, tb)), rrs, self.R("smalls")], [ryn])
            for ci in range(4):
                tt_ = tb * 4 + ci
                i = tt_ % 2
                for half in range(2):
                    b = (tt_ * 2 + half) % 6
                    for cc in range(4):
                        c = half * 4 + cc
                        S.op("pe", "transpose", reads=[ryn, self.R("consts")], writes=[self.PB(b)],
                             out=self.bank(b)[:, cc * 128:(cc + 1) * 128], in_=yn[:, c, ci * 128:(ci + 1) * 128], identity=ident)
                    self.copy(st[i][:, half * 512:(half + 1) * 512], self.bank(b), [self.PB(b)], [rst[i]], eng="act" if half else "dve")
                toks.append(S.dma("sp", self.d["out"][tt_ * 128:(tt_ + 1) * 128, :], st[i], reads=[rst[i]]))
        if not self.recording:
            S.wait_tokens("sp", toks)

    def build(self, nsmall, stage=None):
        self.nsmall = nsmall
        self.stage = stage
        self.setup()
        self.prologue()
        early = None
        for l in range(self.nl):
            early = self.layer(l)
            if early:
                break
        if early:
            kind, view = early
            S = self.S
            S.barrier()
            toks = []
            if kind == "f32":
                for c in range(NCH):
                    toks.append(S.dma("sp", self.dbg_out["dump"][:, c, :], view[:, c, :]))
            else:
                stg = [self.fsq0[:, :], self.fsq1[:, :]]
                rstg = [Res(), Res()]
                for c in range(NCH):
                    for tb in range(NTB):
                        i = tb % 2
                        sl = slice(tb * TB, (tb + 1) * TB)
                        self.copy(stg[i], view[:, c, sl], [], [rstg[i]])
                        toks.append(S.dma("sp", self.dbg_out["dump"][:, c, sl], stg[i], reads=[rstg[i]]))
            S.wait_tokens("sp", toks)
        else:
            self.epilogue()
        if not self.recording:
            self.S.emit()
        return self.nc


def make_program(nsmall, nlayers=L, debug=None, stage=None):
    g0 = Gen(nlayers, debug, record=None)
    g0.build(nsmall, stage)
    g = Gen(nlayers, debug, record=g0.wspecs)
    nc = g.build(nsmall, stage)
    return nc, g


def prepare_inputs(inputs):
    smalls = _build_smalls(inputs)
    bd = np.zeros((L, 8, 128, 8, 128), np.float32)
    bd[:, 0] = _blockdiag(inputs["lru_w_r"])
    bd[:, 1] = _blockdiag(inputs["lru_w_i"])
    bd[:, 2] = _blockdiag(inputs["m_w_q"])
    bd[:, 3] = _blockdiag(inputs["m_w_k"])
    bd[:, 4] = _blockdiag(inputs["m_w_v"])
    bd[:, 5] = _blockdiag(inputs["m_w_q"], transpose=True)
    bd[:, 6] = _blockdiag(inputs["m_w_k"], transpose=True)
    bd[:, 7] = _blockdiag(inputs["m_w_v"], transpose=True)
    shared = {"consts": CONSTS, "smalls": smalls, "bd": bd}
    for n in ("w_ada", "w_in", "w_br_ret", "w_br_lru", "w_br_mlstm", "w_out", "w_ff1", "w_ff2"):
        shared[n] = np.ascontiguousarray(np.asarray(inputs[n], np.float32))
    in_maps = []
    x = np.asarray(inputs["x"], np.float32)
    c = np.asarray(inputs["c"], np.float32)
    pos = np.asarray(inputs["positions"], np.int32)
    for b in range(8):
        m = dict(shared)
        m["x"] = np.ascontiguousarray(x[b])
        m["cvec"] = np.ascontiguousarray(c[b].reshape(8, 128).T)
        m["pos"] = np.ascontiguousarray(pos[b])
        in_maps.append(m)
    return in_maps, smalls.shape[1]


def kernel(**inputs):
    in_maps, nsmall = prepare_inputs(inputs)
    nc, _ = make_program(nsmall)
    res = run_bass_kernel_spmd(nc, in_maps, core_ids=list(range(8)))
    out = np.stack([np.asarray(r["out"], np.float32) for r in res.results], axis=0)
    return out
```

```python
import math
import os
import numpy as np
import concourse.bass as bass
import concourse.mybir as mybir
from concourse.bass_utils import run_bass_kernel_spmd

F32 = mybir.dt.float32
BF16 = mybir.dt.bfloat16
I32 = mybir.dt.int32
AF = mybir.ActivationFunctionType
ALU = mybir.AluOpType
AX = mybir.AxisListType

D = 1024
SEQ = 2048
L = 4
NCH = 8
TB = 512
NTB = 4
D_IN = 10240
EPS = 1e-6
OFF_RQ, OFF_RK, OFF_RV, OFF_RG, OFF_LX, OFF_LY, OFF_MX, OFF_MO, OFF_GATE = 0, 512, 1024, 2048, 3072, 4096, 5120, 6144, 7168
GAMMA = [1.0 - 2.0 ** (-5.0 - h) for h in range(4)]
NB_RING = 4


class Res:
    __slots__ = ("w", "r", "excl", "wn")

    def __init__(self, excl=False):
        self.w = None
        self.wn = 0
        self.r = {}
        self.excl = excl


class Sched:
    ENG = ("pe", "act", "dve", "pool", "sp")
    SAME_N = 256
    STRICT = bool(int(os.environ.get("K_STRICT_SYNC", "0")))
    EPOCH = 8000

    def __init__(self, nc, n_dma_sems=8, record_only=False):
        self.nc = nc
        self.rec = record_only
        self.sems = {}
        self.cnt = {}
        self.ekey = {}
        self.dsem = {}
        self.dcur = {}
        for e in self.ENG:
            self._new_epoch(e, 0)
        for q in ("sp", "pool"):
            self.dsem[q] = ["d_%s%d" % (q, i) for i in range(n_dma_sems)]
            self.dcur[q] = 0
            for k in self.dsem[q]:
                self.cnt[k] = 0
                if not record_only:
                    self.sems[k] = nc.alloc_semaphore("s_" + k)
        self.waited = {e: {} for e in self.ENG}
        self.ops = {e: [] for e in self.ENG}
        self.nops = 0

    def _new_epoch(self, e, idx):
        k = "%s#%d" % (e, idx)
        self.ekey[e] = k
        self.cnt[k] = 0
        if not self.rec:
            self.sems[k] = self.nc.alloc_semaphore("s_%s_%d" % (e, idx))

    def _deps(self, eng, reads, writes):
        need = {}
        pfx = eng + "#"
        for r in reads:
            if r.w is not None:
                k, v = r.w
                if (self.STRICT or not (k.startswith(pfx) and r.wn >= self.SAME_N)) and need.get(k, 0) < v:
                    need[k] = v
            if r.excl:
                for k, v in r.r.items():
                    if not k.startswith(pfx) and need.get(k, 0) < v:
                        need[k] = v
        for r in writes:
            if r.w is not None:
                k, v = r.w
                if (self.STRICT or not k.startswith(pfx)) and need.get(k, 0) < v:
                    need[k] = v
            for k, v in r.r.items():
                if (self.STRICT or not k.startswith(pfx)) and need.get(k, 0) < v:
                    need[k] = v
        out = []
        wd = self.waited[eng]
        for k, v in need.items():
            if eng == "pe" and k.startswith("pe#"):
                continue
            if wd.get(k, 0) < v:
                wd[k] = v
                out.append((k, v))
        return out

    def _mark(self, me, reads, writes, n=0):
        k, v = me
        for r in reads:
            if r.r.get(k, 0) < v:
                r.r[k] = v
        for r in writes:
            r.w = me
            r.wn = n
            r.r = {}

    def op(self, eng, meth, reads=(), writes=(), **kw):
        if self.rec:
            return None
        waits = self._deps(eng, reads, writes)
        k = self.ekey[eng]
        if self.cnt[k] >= self.EPOCH:
            self._new_epoch(eng, int(k.split("#")[1]) + 1)
            k = self.ekey[eng]
        self.cnt[k] += 1
        me = (k, self.cnt[k])
        self.ops[eng].append((waits, meth, kw, (k, 1)))
        o = kw.get("out", kw.get("ap"))
        try:
            n = int(o.free_size()) if o is not None else 0
        except Exception:
            n = 0
        self._mark(me, reads, writes, n)
        self.nops += 1
        return me

    def dma(self, q, out, in_, reads=(), writes=(), **kw):
        if self.rec:
            return None
        lst = self.dsem[q]
        k = lst[self.dcur[q] % len(lst)]
        self.dcur[q] += 1
        waits = self._deps(q, reads, writes)
        if self.cnt[k] > 0 and self.waited[q].get(k, 0) < self.cnt[k]:
            self.waited[q][k] = self.cnt[k]
            waits.append((k, self.cnt[k]))
        self.cnt[k] += 16
        me = (k, self.cnt[k])
        kw = dict(kw)
        kw["out"] = out
        kw["in_"] = in_
        self.ops[q].append((waits, "dma_start", kw, (k, 16)))
        self._mark(me, reads, writes)
        return me

    def barrier(self, engines=("pe", "act", "dve", "sp"), dma=True):
        if self.rec:
            return
        for e in engines:
            if e == "pe":
                continue
            waits = []
            for k in [self.ekey[o] for o in engines if o != e] + (self.dsem["sp"] if dma else []):
                v = self.cnt[k]
                if v > 0 and self.waited[e].get(k, 0) < v:
                    self.waited[e][k] = v
                    waits.append((k, v))
            if waits:
                self.ops[e].append((waits, None, None, None))

    def wait_tokens(self, eng, tokens):
        if self.rec:
            return
        waits = []
        for k, v in tokens:
            if self.waited[eng].get(k, 0) < v:
                self.waited[eng][k] = v
                waits.append((k, v))
        if waits:
            self.ops[eng].append((waits, None, None, None))

    def emit(self):
        sems = self.sems
        ops = self.ops

        def run(e):
            def body(h):
                for waits, meth, kw, inc in ops[e]:
                    for k, v in waits:
                        h.wait_ge(sems[k], v)
                    if meth is not None:
                        ins = getattr(h, meth)(**kw)
                        ins.then_inc(sems[inc[0]], inc[1])
            return body
        with self.nc.Block() as block:
            block.tensor(run("pe"))
            block.scalar(run("act"))
            block.vector(run("dve"))
            block.gpsimd(run("pool"))
            block.sync(run("sp"))


CONST_COLS = {}


def _build_consts():
    cols = []
    off = 0

    def add(name, arr):
        nonlocal off
        a = np.zeros((128, arr.shape[1]), np.float32)
        a[:arr.shape[0]] = arr
        CONST_COLS[name] = (off, arr.shape[1])
        off += arr.shape[1]
        cols.append(a)
    add("ident", np.eye(128, dtype=np.float32))
    p = np.arange(128)
    add("invfreq", (10000.0 ** (-(p % 64).astype(np.float32) / 64.0)).astype(np.float32)[:, None])
    jj = p[:, None].astype(np.float64)
    ii = p[None, :].astype(np.float64)
    for h in range(4):
        m = np.where(ii >= jj, GAMMA[h] ** np.maximum(ii - jj, 0.0), 0.0) / math.sqrt(128.0)
        add("retmask%d" % h, m.astype(np.float32))
    for h in range(4):
        add("dq%d" % h, np.broadcast_to((GAMMA[h] ** (ii + 1.0)), (128, 128)).astype(np.float32))
    kd = np.stack([GAMMA[h] ** (127.0 - p) / math.sqrt(128.0) for h in range(4)], axis=1)
    add("kdscale", kd.astype(np.float32))
    add("causal16", (np.where(ii >= jj, 1.0, 0.0) / 16.0).astype(np.float32))
    sel = np.zeros((4, 512), np.float32)
    for h in range(4):
        sel[h, h * 128:(h + 1) * 128] = 1.0
    add("sel", sel)
    add("ones4", np.ones((4, 128), np.float32))
    add("eye4", np.eye(4, dtype=np.float32))
    rr = np.ones((4, 128), np.float32)
    rr[:, 0] = 0.0
    add("resetrow", rr)
    return np.concatenate(cols, axis=1)


CONSTS = _build_consts()
NCONST = CONSTS.shape[1]

SMALL_COLS = {}


def _fm(v):
    v = np.asarray(v, np.float32)
    lead = v.shape[:-1]
    nch = v.shape[-1] // 128
    a = v.reshape(lead + (nch, 128))
    a = np.moveaxis(a, -1, 0)
    return np.ascontiguousarray(a).reshape(128, -1)


def _build_smalls(inp):
    cols = []
    off = 0

    def add(name, arr):
        nonlocal off
        SMALL_COLS[name] = (off, arr.shape[1])
        off += arr.shape[1]
        cols.append(arr.astype(np.float32))
    add("b_ada", _fm(inp["b_ada"]))
    add("norm_mix", _fm(inp["norm_mix"]))
    add("norm_mlp", _fm(inp["norm_mlp"]))
    add("lru_conv_w", _fm(inp["lru_conv_w"]))
    add("lru_conv_b", _fm(inp["lru_conv_b"]))
    add("lru_b_r", _fm(inp["lru_b_r"]))
    add("lru_b_i", _fm(inp["lru_b_i"]))
    add("lru_lambda", _fm(inp["lru_lambda"]))
    add("m_conv_w", _fm(inp["m_conv_w"]))
    add("m_conv_b", _fm(inp["m_conv_b"]))
    add("m_norm", _fm(inp["m_norm"]))
    add("final_norm", _fm(inp["final_norm"]))
    wif = np.asarray(inp["m_w_if"], np.float32).reshape(L, 24, 128, 8).transpose(2, 0, 1, 3).reshape(128, L * 24 * 8)
    add("m_w_if", wif)
    bif = np.zeros((128, L), np.float32)
    bif[0:8, :] = np.asarray(inp["m_b_if"], np.float32).T
    add("m_b_if", bif)
    return np.ascontiguousarray(np.concatenate(cols, axis=1))


def _blockdiag(w, transpose=False):
    w = np.asarray(w, np.float32)
    Lw, nb, bs, bo = w.shape
    full = np.zeros((Lw, nb * bs, nb * bo), np.float32)
    for n in range(nb):
        blk = w[:, n]
        if transpose:
            blk = blk.transpose(0, 2, 1)
        full[:, n * bs:(n + 1) * bs, n * bo:(n + 1) * bo] = blk
    out = np.zeros((Lw, 128, 8, 128), np.float32)
    for c in range(8):
        out[:, :, c, :] = full[:, c * 128:(c + 1) * 128, c * 128:(c + 1) * 128]
    return out


class Gen:
    def __init__(self, nlayers=L, debug=None, record=None):
        self.nl = nlayers
        self.debug = debug or []
        self.wlist = record
        self.recording = record is None
        self.wspecs = []
        self.widx = 0
        self.wissued = 0
        self.res = {}
        self.ybump = 0

    def R(self, key):
        r = self.res.get(key)
        if r is None:
            r = Res()
            self.res[key] = r
        return r

    def setup(self):
        nc = bass.Bass("TRN2", target_bir_lowering=False)
        self.nc = nc
        self.S = Sched(nc, record_only=self.recording)
        dt = nc.dram_tensor
        self.d = {}
        self.d["x"] = dt("x", [SEQ, D], F32, kind="ExternalInput").ap()
        self.d["cvec"] = dt("cvec", [128, 8], F32, kind="ExternalInput").ap()
        self.d["pos"] = dt("pos", [SEQ], I32, kind="ExternalInput").ap()
        self.d["consts"] = dt("consts", [128, NCONST], F32, kind="ExternalInput").ap()
        self.d["smalls"] = dt("smalls", [128, self.nsmall], F32, kind="ExternalInput").ap()
        self.d["bd"] = dt("bd", [L, 8, 128, 8, 128], F32, kind="ExternalInput").ap()
        self.d["w_ada"] = dt("w_ada", [L, D, 6 * D], F32, kind="ExternalInput").ap()
        self.d["w_in"] = dt("w_in", [L, D, D_IN], F32, kind="ExternalInput").ap()
        for n in ("w_br_ret", "w_br_lru", "w_br_mlstm", "w_out"):
            self.d[n] = dt(n, [L, D, D], F32, kind="ExternalInput").ap()
        self.d["w_ff1"] = dt("w_ff1", [L, D, 4 * D], F32, kind="ExternalInput").ap()
        self.d["w_ff2"] = dt("w_ff2", [L, 4 * D, D], F32, kind="ExternalInput").ap()
        self.d["out"] = dt("out", [SEQ, D], F32, kind="ExternalOutput").ap()
        self.d["xs"] = dt("xs_scratch", [128, NCH, SEQ], F32).ap()
        self.dbg_out = {}
        for name, shape in self.debug:
            self.dbg_out[name] = dt("dbg_" + name, list(shape), F32, kind="ExternalOutput").ap()
        a = nc.alloc_sbuf_tensor
        self.RA = a("RA", [128, 32768], BF16)
        self.RB = a("RB", [128, 32768], BF16)
        self.RM = a("RM", [128, 16384], BF16)
        self.ring = [a("ring%d" % i, [128, 8, 256], BF16) for i in range(NB_RING)]
        self.consts = a("consts_sb", [128, NCONST], F32)
        self.smalls = a("smalls_sb", [128, self.nsmall], F32)
        self.bdw = a("bdw", [128, 5, 8, 128], BF16)
        self.onesb = a("onesb", [128, 128], BF16)
        self.condb = a("condb", [128, 8], BF16)
        self.modv = a("modv", [128, 48], F32)
        self.modn = a("modn", [128, 48], F32)
        self.vecs = a("vecs", [128, 8, 8], F32)
        self.fsq0 = a("fsq0", [128, TB], F32)
        self.fsq1 = a("fsq1", [128, TB], F32)
        self.ps = nc.alloc_psum_tensor("ps", [128, 4096], F32)
        self.X = self.RA
        self.Y = self.RB

    def C(self, name):
        o, n = CONST_COLS[name]
        return self.consts[:, o:o + n]

    def SM(self, name, l=None, per=None):
        o, n = SMALL_COLS[name]
        if l is None:
            return self.smalls[:, o:o + n]
        return self.smalls[:, o + l * per:o + (l + 1) * per]

    def bank(self, i):
        return self.ps[:, i * 512:(i + 1) * 512]

    def PB(self, i):
        r = self.res.get(("ps", i))
        if r is None:
            r = Res(excl=True)
            self.res[("ps", i)] = r
        return r

    def xview(self, reg):
        return reg[:, :].bitcast(F32).rearrange("p (c t) -> p c t", c=NCH)

    def hview(self, reg, off=0):
        return reg[:, off:off + 16384].rearrange("p (c t) -> p c t", c=NCH)

    def ytemp(self, n_elems, dtype):
        sz = n_elems * (2 if dtype == F32 else 1)
        if self.ybump % 2:
            self.ybump += 1
        o = self.ybump
        self.ybump += sz
        assert self.ybump <= 32768, "Y temp overflow %d" % self.ybump
        v = self.Y[:, o:o + sz]
        if dtype == F32:
            v = v.bitcast(F32)
        return v

    def wget(self, name, l, kg, c0, ncols):
        spec = (name, l, kg, c0, ncols)
        if self.recording:
            self.wspecs.append(spec)
            return self.ring[0][:, :, 0:ncols], self.R(("ring", 0))
        i = self.widx
        assert self.wlist[i] == spec, (i, self.wlist[i], spec)
        self.widx += 1
        while self.wissued < len(self.wlist) and self.wissued <= i + NB_RING - 1:
            j = self.wissued
            n2, l2, kg2, c2, nc2 = self.wlist[j]
            slot = j % NB_RING
            src = self.d[n2][l2, kg2 * 1024:(kg2 + 1) * 1024, c2:c2 + nc2].rearrange("(k p) n -> p k n", p=128)
            self.S.dma("pool", self.ring[slot][:, :, 0:nc2], src, writes=[self.R(("ring", slot))])
            self.wissued += 1
        slot = i % NB_RING
        return self.ring[slot][:, :, 0:ncols], self.R(("ring", slot))

    def mm(self, out, lhsT, rhs, start, stop, reads, writes):
        self.S.op("pe", "matmul", reads=reads, writes=writes, out=out, lhsT=lhsT, rhs=rhs, start=start, stop=stop)

    def act(self, out, in_, func, reads, writes, scale=1.0, bias=0.0):
        kw = dict(out=out, in_=in_, func=func)
        if not (isinstance(scale, float) and scale == 1.0):
            kw["scale"] = scale
        if not (isinstance(bias, float) and bias == 0.0):
            kw["bias"] = bias
        self.S.op("act", "activation", reads=reads, writes=writes, **kw)

    def tt(self, out, in0, in1, op, reads, writes, eng="dve"):
        self.S.op(eng, "tensor_tensor", reads=reads, writes=writes, out=out, in0=in0, in1=in1, op=op)

    def ts(self, out, in0, s1, s2, op0, op1, reads, writes, eng="dve"):
        if s2 is None:
            self.S.op(eng, "tensor_scalar", reads=reads, writes=writes, out=out, in0=in0, scalar1=s1, scalar2=None, op0=op0)
        else:
            self.S.op(eng, "tensor_scalar", reads=reads, writes=writes, out=out, in0=in0, scalar1=s1, scalar2=s2, op0=op0, op1=op1)

    def stt(self, out, in0, scalar, in1, op0, op1, reads, writes, eng="dve"):
        self.S.op(eng, "scalar_tensor_tensor", reads=reads, writes=writes, out=out, in0=in0, scalar=scalar, in1=in1, op0=op0, op1=op1)

    def copy(self, out, in_, reads, writes, eng="dve"):
        if eng == "act":
            self.act(out, in_, AF.Copy, reads, writes)
        else:
            self.S.op(eng, "tensor_copy", reads=reads, writes=writes, out=out, in_=in_)

    def dump(self, name, ap, reads):
        if name in self.dbg_out:
            self.S.barrier()
            t = self.S.dma("sp", self.dbg_out[name], ap, reads=reads)
            self.S.barrier()

    def proj_fm(self, blk, rblk, coff, hsrc, hkey, banks, kchunks=NCH):
        for tb in range(NTB):
            b = banks[tb]
            for k in range(kchunks):
                self.mm(self.bank(b), blk[:, k, coff:coff + 128], hsrc[:, k, tb * TB:(tb + 1) * TB],
                        k == 0, k == kchunks - 1, [rblk, self.R((hkey, tb))], [self.PB(b)])

    def rstd_from_bank(self, b, out, n, rout):
        self.ts(out, self.bank(b), 1.0 / n, EPS, ALU.mult, ALU.add, [self.PB(b)], [rout])
        self.act(out, out, AF.Sqrt, [rout], [rout])
        self.S.op("dve", "reciprocal", reads=[rout], writes=[rout], out=out, in_=out)

    def norm(self, xv, xkey, Av, Bv, dst, dkey, tmp_pool):
        sq, rsq, rs, rrs, tmp, rtmp = tmp_pool
        for tb in range(NTB):
            sl = slice(tb * TB, (tb + 1) * TB)
            b = 4 + tb
            for c in range(NCH):
                i = (tb * NCH + c) % 2
                self.act(sq[i], xv[:, c, sl], AF.Square, [self.R((xkey, c, tb))], [rsq[i]])
                self.mm(self.bank(b), self.onesb[:, :], sq[i], c == 0, c == NCH - 1, [rsq[i], self.R("onesb")], [self.PB(b)])
        for tb in range(NTB):
            self.rstd_from_bank(4 + tb, rs[tb], float(D), rrs[tb])
        for tb in range(NTB):
            sl = slice(tb * TB, (tb + 1) * TB)
            for c in range(NCH):
                i = (tb * NCH + c) % 2
                self.tt(tmp[i], xv[:, c, sl], rs[tb], ALU.mult, [self.R((xkey, c, tb)), rrs[tb]], [rtmp[i]])
                self.act(dst[:, c, sl], tmp[i], AF.Identity, [rtmp[i], self.R("vecs")], [self.R((dkey, tb))],
                         scale=Av[:, c:c + 1], bias=Bv[:, c:c + 1])

    def norm_tmps(self, alloc):
        sq = [alloc(TB, BF16) for _ in range(2)]
        rs = [alloc(TB, F32) for _ in range(NTB)]
        tmp = [alloc(TB, F32) for _ in range(2)]
        return (sq, [Res(), Res()], rs, [Res() for _ in range(NTB)], tmp, [Res(), Res()])

    def prologue(self):
        S = self.S
        S.dma("sp", self.consts[:, :], self.d["consts"], writes=[self.R("consts")])
        S.dma("sp", self.smalls[:, :], self.d["smalls"], writes=[self.R("smalls")])
        self.ybump = 0
        cf = self.ytemp(8, F32)
        rcf = Res()
        S.dma("sp", cf, self.d["cvec"], writes=[rcf])
        self.act(self.condb[:, :], cf, AF.Silu, [rcf], [self.R("condb")])
        S.op("dve", "memset", reads=[], writes=[self.R("onesb")], ap=self.onesb[:, :], constant=1.0)
        S.barrier()
        self.ybump = 0
        xs = [self.ytemp(D, F32) for _ in range(2)]
        rxs = [Res(), Res()]
        xv = self.xview(self.X)
        ident = self.C("ident")
        for tt_ in range(16):
            i = tt_ % 2
            S.dma("sp", xs[i], self.d["x"][tt_ * 128:(tt_ + 1) * 128, :], writes=[rxs[i]])
            tb = tt_ // 4
            for half in range(2):
                b = (tt_ * 2 + half) % 8
                for cc in range(4):
                    c = half * 4 + cc
                    S.op("pe", "transpose", reads=[rxs[i], self.R("consts")], writes=[self.PB(b)],
                         out=self.bank(b)[:, cc * 128:(cc + 1) * 128], in_=xs[i][:, c * 128:(c + 1) * 128], identity=ident)
                eng = "act" if half == 0 else "dve"
                self.copy(xv[:, half * 4:(half + 1) * 4, tt_ * 128:(tt_ + 1) * 128],
                          self.bank(b).rearrange("p (c t) -> p c t", c=4), [self.PB(b)],
                          [self.R(("x", half * 4 + cc, tb)) for cc in range(4)], eng=eng)
        S.barrier()

    def rotary_tables(self):
        S = self.S
        self.cosT = self.ytemp(SEQ, BF16)
        self.sinT = self.ytemp(SEQ, BF16)
        keep = self.ybump
        posi = self.ytemp(SEQ, F32).bitcast(I32)
        rpos = Res()
        S.dma("sp", posi, self.d["pos"].partition_broadcast(128), writes=[rpos])
        ang = self.ytemp(TB, F32)
        u = self.ytemp(TB, F32)
        ki = self.ytemp(TB, F32).bitcast(I32)
        kf = self.ytemp(TB, F32)
        ru = Res()
        for tb in range(NTB):
            sl = slice(tb * TB, (tb + 1) * TB)
            self.copy(ang, posi[:, sl], [rpos, ru], [ru])
            self.ts(ang, ang, self.C("invfreq"), None, ALU.mult, None, [ru, self.R("consts")], [ru])
            for dst, shift in ((self.sinT, math.pi), (self.cosT, 1.5 * math.pi)):
                self.ts(u, ang, shift, None, ALU.add, None, [ru], [ru])
                self.ts(ki, u, 1.0 / (2 * math.pi), None, ALU.mult, None, [ru], [ru])
                self.copy(kf, ki, [ru], [ru])
                self.stt(u, kf, -2 * math.pi, u, ALU.mult, ALU.add, [ru], [ru])
                self.ts(kf, u, 0.0, 2 * math.pi, ALU.is_lt, ALU.mult, [ru], [ru])
                self.tt(u, u, kf, ALU.add, [ru], [ru])
                self.ts(u, u, -math.pi, math.pi, ALU.add, ALU.min, [ru], [ru])
                self.ts(u, u, -math.pi, None, ALU.max, None, [ru], [ru])
                self.act(dst[:, sl], u, AF.Sin, [ru], [self.R("rot")])
        S.barrier()
        self.ybump = keep

    def adaln_block(self, l, blk_i, b, dst, rdst):
        blk, rblk = self.wget("w_ada", l, 0, blk_i * 256, 256)
        for fc in range(2):
            for k in range(NCH):
                self.mm(self.bank(b)[:, fc:fc + 1], blk[:, k, fc * 128:(fc + 1) * 128], self.condb[:, k:k + 1],
                        k == 0, k == NCH - 1, [rblk, self.R("condb")], [self.PB(b)])
        self.copy(dst[:, blk_i * 2:blk_i * 2 + 2], self.bank(b)[:, 0:2], [self.PB(b)], [rdst])

    def adaln(self, l, precomputed=False):
        S = self.S
        rv = self.R("vecs")
        if not precomputed:
            for blk_i in range(24):
                self.adaln_block(l, blk_i, 7, self.modn, self.R("modn"))
        self.tt(self.modv[:, :], self.modn[:, :], self.SM("b_ada", l, 48), ALU.add, [self.R("modn"), self.R("smalls"), rv], [rv])
        self.stt(self.vecs[:, 0, :], self.modv[:, 8:16], 1.0, self.SM("norm_mix", l, 8), ALU.add, ALU.mult, [rv, self.R("smalls")], [rv])
        self.stt(self.vecs[:, 1, :], self.modv[:, 32:40], 1.0, self.SM("norm_mlp", l, 8), ALU.add, ALU.mult, [rv, self.R("smalls")], [rv])
        self.act(self.vecs[:, 4, :], self.SM("lru_lambda", l, 8), AF.Exp, [self.R("smalls"), rv], [rv], scale=-1.0)
        self.act(self.vecs[:, 4, :], self.vecs[:, 4, :], AF.Ln, [rv], [rv], bias=1.0)
        self.ts(self.vecs[:, 2, :], self.vecs[:, 4, :], -8.0, None, ALU.mult, None, [rv], [rv])
        self.ts(self.vecs[:, 3, :], self.vecs[:, 4, :], -16.0, None, ALU.mult, None, [rv], [rv])
        for kind in range(5):
            S.dma("pool", self.bdw[:, kind, :, :], self.d["bd"][l, kind], writes=[self.R("bdw")])

    def branch_merge(self, l, bidx, wname):
        bo = self.hview(self.X, 0)
        mg = self.hview(self.X, 16384)
        hT = self.hview(self.RM)
        gsb = [self.ytemp(SEQ, BF16) for _ in range(2)]
        rg = [Res(), Res()]
        tmpm = [self.ytemp(TB, BF16) for _ in range(2)]
        rtm = [Res(), Res()]
        for f in range(NCH):
            gi = f % 2
            blk, rblk = self.wget("w_in", l, 0, OFF_GATE + bidx * 1024 + f * 128, 128)
            self.proj_fm(blk, rblk, 0, hT, "h", [0, 1, 2, 3])
            for tb in range(NTB):
                self.act(gsb[gi][:, tb * TB:(tb + 1) * TB], self.bank(tb), AF.Sigmoid, [self.PB(tb)], [rg[gi]])
            wblk, rwblk = self.wget(wname, l, 0, f * 128, 128)
            self.proj_fm(wblk, rwblk, 0, bo, "bo", [4, 5, 6, 7])
            for tb in range(NTB):
                sl = slice(tb * TB, (tb + 1) * TB)
                if bidx == 0:
                    self.tt(mg[:, f, sl], self.bank(4 + tb), gsb[gi][:, sl], ALU.mult, [self.PB(4 + tb), rg[gi]], [self.R(("mg", tb))])
                else:
                    i = tb % 2
                    self.tt(tmpm[i], self.bank(4 + tb), gsb[gi][:, sl], ALU.mult, [self.PB(4 + tb), rg[gi]], [rtm[i]])
                    self.tt(mg[:, f, sl], mg[:, f, sl], tmpm[i], ALU.add, [rtm[i], self.R(("mg", tb))], [self.R(("mg", tb))])

    def lru_branch(self, l):
        S = self.S
        self.ybump = 0
        bo = self.hview(self.X, 0)
        hT = self.hview(self.RM)
        xl = self.ytemp(SEQ, F32)
        a = self.ytemp(SEQ, F32)
        u = self.ytemp(SEQ, F32)
        rsb = self.ytemp(SEQ, F32)
        isb = self.ytemp(SEQ, F32)
        t1 = self.ytemp(SEQ, F32)
        xlb = self.ytemp(SEQ, BF16)
        rxl, ra, ru, rxlb, rrs, ris, rt1 = Res(), Res(), Res(), Res(), Res(), Res(), Res()
        rsm = self.R("smalls")
        rv = self.R("vecs")
        A_lo = self.ps[:, 0:SEQ]
        A_hi = self.ps[:, SEQ:2 * SEQ]
        lo = [self.PB(i) for i in range(4)]
        hi = [self.PB(4 + i) for i in range(4)]
        cw = self.SM("lru_conv_w", l, 32)
        cb = self.SM("lru_conv_b", l, 8)
        for c in range(NCH):
            blk, rblk = self.wget("w_in", l, 0, OFF_LX + c * 128, 128)
            self.proj_fm(blk, rblk, 0, hT, "h", [0, 1, 2, 3])
            self.ts(xl, A_lo, cw[:, 3 * 8 + c:3 * 8 + c + 1], cb[:, c:c + 1], ALU.mult, ALU.add, lo + [rsm], [rxl])
            for s_ in range(1, 4):
                self.stt(xl[:, s_:SEQ], A_lo[:, 0:SEQ - s_], cw[:, (3 - s_) * 8 + c:(3 - s_) * 8 + c + 1], xl[:, s_:SEQ],
                         ALU.mult, ALU.add, lo + [rsm, rxl], [rxl])
            self.copy(xlb, xl, [rxl], [rxlb], eng="act")
            for tb in range(NTB):
                sl = slice(tb * TB, (tb + 1) * TB)
                self.mm(self.bank(4 + tb), self.bdw[:, 0, c, :], xlb[:, sl], True, True, [rxlb, self.R("bdw")], [self.PB(4 + tb)])
            for tb in range(NTB):
                sl = slice(tb * TB, (tb + 1) * TB)
                self.mm(self.bank(tb), self.bdw[:, 1, c, :], xlb[:, sl], True, True, [rxlb, self.R("bdw")], [self.PB(tb)])
            self.act(rsb, A_hi, AF.Sigmoid, hi + [rsm], [rrs], bias=self.SM("lru_b_r", l, 8)[:, c:c + 1])
            self.act(isb, A_lo, AF.Sigmoid, lo + [rsm], [ris], bias=self.SM("lru_b_i", l, 8)[:, c:c + 1])
            blk, rblk = self.wget("w_in", l, 0, OFF_LY + c * 128, 128)
            self.proj_fm(blk, rblk, 0, hT, "h", [4, 5, 6, 7])
            self.act(a, rsb, AF.Exp, [rrs, rv], [ra], scale=self.vecs[:, 2, c:c + 1])
            self.act(t1, rsb, AF.Exp, [rrs, rv], [rt1], scale=self.vecs[:, 3, c:c + 1])
            self.ts(t1, t1, -1.0, 1.0, ALU.mult, ALU.add, [rt1], [rt1])
            self.act(t1, t1, AF.Sqrt, [rt1], [rt1])
            self.tt(u, isb, xl, ALU.mult, [ris, rxl], [ru])
            self.tt(u, u, t1, ALU.mult, [ru, rt1], [ru])
            S.op("dve", "tensor_tensor_scan", reads=[ra, ru, rxl], writes=[rxl], out=xl, data0=a, data1=u, initial=0.0,
                 op0=ALU.mult, op1=ALU.add)
            self.act(t1, A_hi, AF.Square, hi + [rt1], [rt1])
            self.ts(t1, t1, 0.0713548163, 1.5957691216, ALU.mult, ALU.add, [rt1], [rt1])
            self.tt(t1, t1, A_hi, ALU.mult, [rt1] + hi, [rt1])
            self.act(t1, t1, AF.Sigmoid, [rt1], [rt1])
            self.tt(t1, t1, A_hi, ALU.mult, [rt1] + hi, [rt1])
            self.tt(bo[:, c, :], t1, xl, ALU.mult, [rt1, rxl], [self.R(("bo", tb)) for tb in range(NTB)])

    def ret_branch(self, l):
        S = self.S
        bo = self.hview(self.X, 0)
        hT = self.hview(self.RM)
        rc = self.R("consts")
        self.ybump = 0
        self.rotary_tables()
        qT = self.ytemp(SEQ, BF16)
        qsT = self.ytemp(SEQ, BF16)
        kT = self.ytemp(SEQ, BF16)
        kd = self.ytemp(16 * 128, BF16).rearrange("p (c d) -> p c d", c=16)
        vt = self.ytemp(16 * 256, BF16).rearrange("p (c e) -> p c e", c=16)
        srg = self.ytemp(2 * SEQ, BF16).rearrange("p (c t) -> p c t", c=2)
        pblk = self.ytemp(8 * 128, BF16).rearrange("p (k n) -> p k n", k=8)
        t1 = [self.ytemp(TB, F32) for _ in range(2)]
        t2 = [self.ytemp(TB, F32) for _ in range(2)]
        Sst2 = [self.ytemp(256, F32) for _ in range(2)]
        Sbf2 = [self.ytemp(256, BF16) for _ in range(2)]
        sT = [self.ytemp(128, BF16) for _ in range(2)]
        sq = self.ytemp(2 * TB, BF16).rearrange("p (c t) -> p c t", c=2)
        rs = self.ytemp(TB, F32)
        tmp = self.ytemp(TB, F32)
        rq, rqs, rk, rkd, rvt, rsrg, rpb = Res(), Res(), Res(), Res(), Res(), Res(), Res()
        rt1, rt2 = [Res(), Res()], [Res(), Res()]
        rS2, rSb2, rsT, rsq, rrs, rtmp = [Res(), Res()], [Res(), Res()], [Res(), Res()], Res(), Res(), Res()
        ident = self.C("ident")
        for h in range(4):
            for which in range(2):
                off = (OFF_RQ if which == 0 else OFF_RK) + h * 128
                blk, rblk = self.wget("w_in", l, 0, off, 128)
                self.act(pblk[:, :, 0:64], blk[:, :, 64:128], AF.Copy, [rblk], [rpb], scale=-1.0)
                self.copy(pblk[:, :, 64:128], blk[:, :, 0:64], [rblk], [rpb], eng="dve")
                self.proj_fm(blk, rblk, 0, hT, "h", [0, 1, 2, 3])
                self.proj_fm(pblk, rpb, 0, hT, "h", [4, 5, 6, 7])
                for tb in range(NTB):
                    sl = slice(tb * TB, (tb + 1) * TB)
                    i = tb % 2
                    self.tt(t1[i], self.bank(tb), self.cosT[:, sl], ALU.mult, [self.PB(tb), self.R("rot")], [rt1[i]])
                    self.tt(t2[i], self.bank(4 + tb), self.sinT[:, sl], ALU.mult, [self.PB(4 + tb), self.R("rot")], [rt2[i]])
                    self.tt(t1[i], t1[i], t2[i], ALU.add, [rt1[i], rt2[i]], [rt1[i]])
                    if which == 0:
                        self.copy(qT[:, sl], t1[i], [rt1[i]], [rq], eng="act")
                        dq = self.C("dq%d" % h).unsqueeze(1).broadcast_to([128, 4, 128])
                        self.tt(qsT[:, sl].rearrange("p (a b) -> p a b", a=4), t1[i].rearrange("p (a b) -> p a b", a=4), dq,
                                ALU.mult, [rt1[i], rc], [rqs])
                    else:
                        self.copy(kT[:, sl], t1[i], [rt1[i]], [rk], eng="act")
                        for cc in range(4):
                            S.op("pe", "transpose", reads=[rt1[i], rc], writes=[self.PB(tb)],
                                 out=self.bank(tb)[:, cc * 128:(cc + 1) * 128], in_=t1[i][:, cc * 128:(cc + 1) * 128], identity=ident)
                        self.ts(kd[:, tb * 4:(tb + 1) * 4, :], self.bank(tb).rearrange("p (c d) -> p c d", c=4),
                                self.C("kdscale")[:, h:h + 1], None, ALU.mult, None, [self.PB(tb), rc], [rkd])
            blk, rblk = self.wget("w_in", l, 0, OFF_RV + h * 256, 256)
            for tt_ in range(16):
                b = tt_ // 2 % 8
                o = (tt_ % 2) * 256
                for k in range(NCH):
                    self.mm(self.bank(b)[:, o:o + 256], hT[:, k, tt_ * 128:(tt_ + 1) * 128], blk[:, k, :], k == 0, k == NCH - 1,
                            [rblk, self.R(("h", tt_ // 4))], [self.PB(b)])
                if tt_ % 2 == 1:
                    self.copy(vt[:, tt_ - 1:tt_ + 1, :], self.bank(b).rearrange("p (c e) -> p c e", c=2), [self.PB(b)], [rvt],
                              eng="act" if (tt_ // 2) % 2 else "dve")
            blk, rblk = self.wget("w_in", l, 0, OFF_RG + h * 256, 256)
            for ec in range(2):
                banks = [0, 1, 2, 3] if ec == 0 else [4, 5, 6, 7]
                self.proj_fm(blk, rblk, ec * 128, hT, "h", banks)
                for tb in range(NTB):
                    self.act(srg[:, ec, tb * TB:(tb + 1) * TB], self.bank(banks[tb]), AF.Silu, [self.PB(banks[tb])], [rsrg])
            g128 = GAMMA[h] ** 128.0
            mask = self.C("retmask%d" % h)
            for c in range(16):
                tb = c // 4
                cs = slice(c * 128, (c + 1) * 128)
                si = c % 2
                bs = si
                ob = [2 + 2 * (tb % 2), 3 + 2 * (tb % 2)]
                oc = slice((c % 4) * 128, (c % 4 + 1) * 128)
                self.mm(self.bank(bs)[:, 0:128], kT[:, cs], qT[:, cs], True, True, [rk, rq], [self.PB(bs)])
                if c < 15:
                    self.mm(self.bank(6)[:, 0:256], kd[:, c, :], vt[:, c, :], True, True, [rkd, rvt], [self.PB(6)])
                self.tt(sT[si], self.bank(bs)[:, 0:128], mask, ALU.mult, [self.PB(bs), rc], [rsT[si]])
                pn, pc = (c + 1) % 2, c % 2
                if c < 15:
                    if c == 0:
                        self.copy(Sst2[pn], self.bank(6)[:, 0:256], [self.PB(6)], [rS2[pn]])
                    else:
                        self.stt(Sst2[pn], Sst2[pc], g128, self.bank(6)[:, 0:256], ALU.mult, ALU.add, [rS2[pc], self.PB(6)], [rS2[pn]])
                    self.copy(Sbf2[pn], Sst2[pn], [rS2[pn]], [rSb2[pn]], eng="act")
                for ec in range(2):
                    es = slice(ec * 128, (ec + 1) * 128)
                    self.mm(self.bank(ob[ec])[:, oc], vt[:, c, es], sT[si], True, c == 0, [rvt, rsT[si]], [self.PB(ob[ec])])
                    if c > 0:
                        self.mm(self.bank(ob[ec])[:, oc], Sbf2[pc][:, es], qsT[:, cs], False, True, [rSb2[pc], rqs], [self.PB(ob[ec])])
                if c % 4 == 3:
                    sl = slice(tb * TB, (tb + 1) * TB)
                    for ec in range(2):
                        self.act(sq[:, ec, :], self.bank(ob[ec]), AF.Square, [self.PB(ob[ec])], [rsq])
                    for ec in range(2):
                        self.mm(self.bank(7), self.onesb[:, :], sq[:, ec, :], ec == 0, ec == 1, [rsq, self.R("onesb")], [self.PB(7)])
                    self.rstd_from_bank(7, rs, 256.0, rrs)
                    for ec in range(2):
                        self.tt(tmp, self.bank(ob[ec]), rs, ALU.mult, [self.PB(ob[ec]), rrs], [rtmp])
                        self.tt(bo[:, 2 * h + ec, sl], tmp, srg[:, ec, sl], ALU.mult, [rtmp, rsrg], [self.R(("bo", tb))])

    def conv_tb(self, A_all, pbs, cw, cb, ch, tb, dst, rdst, rsm):
        t0 = tb * TB
        pbs = [self.PB(tb)] + ([self.PB(tb - 1)] if tb > 0 else [])
        self.ts(dst, A_all[:, t0:t0 + TB], cw[:, 3 * 8 + ch:3 * 8 + ch + 1], cb[:, ch:ch + 1], ALU.mult, ALU.add, [self.PB(tb), rsm], [rdst])
        for s in range(1, 4):
            lo = s if tb == 0 else 0
            self.stt(dst[:, lo:TB], A_all[:, t0 + lo - s:t0 + TB - s], cw[:, (3 - s) * 8 + ch:(3 - s) * 8 + ch + 1], dst[:, lo:TB],
                     ALU.mult, ALU.add, pbs + [rsm, rdst], [rdst])

    def mlstm_branch(self, l):
        S = self.S
        bo = self.hview(self.X, 0)
        hT = self.hview(self.RM)
        rc = self.R("consts")
        rsm = self.R("smalls")
        rbd = self.R("bdw")
        A_all = self.ps[:, 0:SEQ]
        cw = self.SM("m_conv_w", l, 32)
        cb = self.SM("m_conv_b", l, 8)
        ident = self.C("ident")
        self.ybump = 0
        bT = self.ytemp(SEQ, F32)
        decb = self.ytemp(64, F32)
        wkt = self.ytemp(64, F32)
        rb, rdecb, rwkt = Res(), Res(), Res()
        base = self.ybump
        bdT = self.ytemp(3 * NCH * 128, F32).rearrange("p (k c n) -> p k c n", k=3, c=NCH)
        rbdT = Res()
        for kind in range(3):
            S.dma("sp", bdT[:, kind, :, :], self.d["bd"][l, 5 + kind], writes=[rbdT])
        wab = self.ytemp(2 * NCH * 128, BF16).rearrange("p (a c g) -> p a c g", a=2, c=NCH)
        rwab = Res()
        S.op("dve", "memset", reads=[], writes=[rwab], ap=wab, constant=0.0)
        S.op("dve", "memset", reads=[], writes=[rb], ap=bT, constant=0.0)
        wif = self.SM("m_w_if", l, 192).rearrange("p (c g) -> p c g", c=24)
        for c in range(NCH):
            self.mm(self.bank(7)[:, c * 16:c * 16 + 8], bdT[:, 0, c, :], wif[:, c, :], True, False, [rbdT, rsm], [self.PB(7)])
            self.mm(self.bank(7)[:, c * 16:c * 16 + 8], bdT[:, 1, c, :], wif[:, 8 + c, :], False, True, [rbdT, rsm], [self.PB(7)])
            self.mm(self.bank(7)[:, c * 16 + 8:c * 16 + 16], bdT[:, 2, c, :], wif[:, 16 + c, :], True, True, [rbdT, rsm], [self.PB(7)])
        b7 = self.bank(7)[:, 0:128].rearrange("p (c a g) -> p a c g", c=NCH, a=2)
        self.copy(wab[:, :, :, 0:8], b7, [self.PB(7), rwab], [rwab])
        xct = [self.ytemp(TB, F32) for _ in range(2)]
        xcb = [self.ytemp(TB, BF16) for _ in range(2)]
        mxb = [self.ytemp(TB, BF16) for _ in range(2)]
        rxct, rxcb, rmxb = [Res(), Res()], [Res(), Res()], [Res(), Res()]
        for c in range(NCH):
            blk, rblk = self.wget("w_in", l, 0, OFF_MX + c * 128, 128)
            self.proj_fm(blk, rblk, 0, hT, "h", [0, 1, 2, 3])
            pbs = [self.PB(i) for i in range(4)]
            for tb in range(NTB):
                i = tb % 2
                self.copy(mxb[i], self.bank(tb), [self.PB(tb)], [rmxb[i]], eng="act")
                self.conv_tb(A_all, pbs, cw, cb, c, tb, xct[i], rxct[i], rsm)
                self.act(xcb[i], xct[i], AF.Silu, [rxct[i]], [rxcb[i]])
                self.mm(self.bank(4 + tb), wab[:, 0, c, :], xcb[i], c == 0, False, [rwab, rxcb[i]], [self.PB(4 + tb)])
                self.mm(self.bank(4 + tb), wab[:, 1, c, :], mxb[i], False, c == NCH - 1, [rwab, rmxb[i]], [self.PB(4 + tb)])
        S.barrier()
        self.ybump = base
        ifT = self.ytemp(SEQ, F32)
        fT = self.ytemp(SEQ, F32)
        gT = self.ytemp(SEQ, F32)
        sm = self.ytemp(256, F32)
        rif, rf, rgt, rsmr = Res(), Res(), Res(), Res()
        S.op("dve", "memset", reads=[], writes=[rgt], ap=gT, constant=0.0)
        S.op("dve", "memset", reads=[], writes=[rsmr], ap=sm, constant=0.0)
        bif = self.SM("m_b_if", l, 1)
        for tb in range(NTB):
            sl = slice(tb * TB, (tb + 1) * TB)
            self.act(ifT[0:8, sl], self.bank(4 + tb)[0:8, :], AF.Identity, [self.PB(4 + tb), rsm], [rif], bias=bif[0:8, :])
        self.dump("ifpre", ifT[0:8, :], [rif])
        S.dma("sp", fT[0:4, :], ifT[4:8, :], reads=[rif], writes=[rf])
        F4, B4, G4, I4 = fT[0:4, :], bT[0:4, :], gT[0:4, :], ifT[0:4, :]
        self.stt(B4, F4, -1.0, F4, ALU.mult, ALU.max, [rf], [rb])
        self.act(B4, B4, AF.Exp, [rb], [rb], scale=-1.0)
        self.act(B4, B4, AF.Ln, [rb], [rb], bias=1.0)
        self.ts(G4, F4, 0.0, None, ALU.min, None, [rf], [rgt])
        self.tt(F4, G4, B4, ALU.subtract, [rgt, rb], [rf])
        rr = self.C("resetrow")[0:4, :].unsqueeze(1).broadcast_to([4, 16, 128])
        self.copy(G4.rearrange("p (c t) -> p c t", c=16), rr, [rc, rgt], [rgt])
        S.op("dve", "tensor_tensor_scan", reads=[rgt, rf, rb], writes=[rb], out=B4, data0=G4, data1=F4, initial=0.0,
             op0=ALU.mult, op1=ALU.add)
        self.tt(G4, I4, B4, ALU.subtract, [rif, rb, rgt], [rgt])
        Gc = sm[0:4, 0:16]
        bl = sm[0:4, 16:32]
        ms1 = sm[0:4, 32:48]
        ms0 = sm[0:4, 48:64]
        Mc = sm[0:4, 64:80]
        dec = sm[0:4, 80:96]
        S.op("dve", "tensor_reduce", reads=[rgt], writes=[rsmr], out=Gc, in_=G4.rearrange("p (c t) -> p c t", c=16),
             axis=AX.X, op=ALU.max)
        self.copy(bl, B4.rearrange("p (c t) -> p c t", c=16)[:, :, 127], [rb, rsmr], [rsmr])
        S.op("dve", "tensor_tensor_scan", reads=[rsmr], writes=[rsmr], out=ms1, data0=Gc, data1=bl, initial=0.0,
             op0=ALU.max, op1=ALU.add)
        S.op("dve", "memset", reads=[rsmr], writes=[rsmr], ap=ms0[:, 0:1], constant=0.0)
        self.copy(ms0[:, 1:16], ms1[:, 0:15], [rsmr], [rsmr])
        self.tt(Mc, ms0, Gc, ALU.max, [rsmr], [rsmr])
        self.tt(dec, ms0, Mc, ALU.subtract, [rsmr], [rsmr])
        self.act(dec, dec, AF.Exp, [rsmr], [rsmr])
        Mb = Mc.unsqueeze(2).broadcast_to([4, 16, 128])
        self.tt(G4.rearrange("p (c t) -> p c t", c=16), G4.rearrange("p (c t) -> p c t", c=16), Mb, ALU.subtract,
                [rgt, rsmr], [rgt])
        self.act(G4, G4, AF.Exp, [rgt], [rgt])
        self.tt(B4.rearrange("p (c t) -> p c t", c=16), B4.rearrange("p (c t) -> p c t", c=16), Mb, ALU.add,
                [rb, rsmr], [rb])
        dexp = sm[0:4, 96:160].rearrange("p (h c) -> p h c", h=4)
        self.tt(dexp, dec.unsqueeze(1).broadcast_to([4, 4, 16]), self.C("eye4")[0:4, :].unsqueeze(2).broadcast_to([4, 4, 16]),
                ALU.mult, [rsmr, rc], [rsmr])
        self.mm(self.bank(4)[:, 0:64], self.C("ones4"), sm[:, 96:160], True, True, [rsmr, rc], [self.PB(4)])
        self.copy(decb, self.bank(4)[:, 0:64], [self.PB(4)], [rdecb])
        for c in range(16):
            S.op("pe", "transpose", reads=[rgt, rc], writes=[self.PB(c // 4)], out=self.bank(c // 4)[:, (c % 4) * 128:(c % 4 + 1) * 128],
                 in_=gT[:, c * 128:(c + 1) * 128], identity=ident)
        for cb4 in range(4):
            self.copy(wkt[:, cb4 * 16:(cb4 + 1) * 16].rearrange("p (c h) -> p c h", c=4),
                      self.bank(cb4).rearrange("p (c t) -> p c t", c=4)[:, :, 0:4], [self.PB(cb4)], [rwkt])
        S.barrier()
        self.ybump = base
        mxh = self.ytemp(2 * SEQ, BF16).rearrange("p (c t) -> p c t", c=2)
        xch = self.ytemp(2 * SEQ, BF16).rearrange("p (c t) -> p c t", c=2)
        smo = self.ytemp(2 * SEQ, BF16).rearrange("p (c t) -> p c t", c=2)
        xct1 = self.ytemp(TB, F32)
        xct = [xct1, xct1]
        qTb = self.ytemp(2 * TB, BF16).rearrange("p (c t) -> p c t", c=2)
        kTb = self.ytemp(2 * TB, BF16).rearrange("p (c t) -> p c t", c=2)
        kw = self.ytemp(4 * 256, BF16).rearrange("p (c d) -> p c d", c=4)
        vx = self.ytemp(4 * 384, BF16).rearrange("p (c e) -> p c e", c=4)
        Cst2 = [self.ytemp(2 * 384, F32).rearrange("p (c e) -> p c e", c=2) for _ in range(2)]
        Cbf2 = [self.ytemp(2 * 384, BF16).rearrange("p (c e) -> p c e", c=2) for _ in range(2)]
        sT = [self.ytemp(128, BF16) for _ in range(2)]
        thb = self.ytemp(TB, F32)
        rcp = thb
        hm = self.ytemp(2 * TB, F32).rearrange("p (c t) -> p c t", c=2)
        sq = self.ytemp(2 * TB, BF16).rearrange("p (c t) -> p c t", c=2)
        rs = self.ytemp(TB, F32)
        rmxh, rxch, rqT, rkT, rkw, rvx, rsmo = Res(), Res(), Res(), Res(), Res(), Res(), Res()
        rC2, rCb2 = [Res(), Res()], [Res(), Res()]
        rsT, rthb, rhm, rsq, rrs = [Res(), Res()], Res(), Res(), Res(), Res()
        rrcp = rthb
        rx1 = Res()
        rxct = [rx1, rx1]
        S.op("dve", "memset", reads=[], writes=[rvx], ap=vx[:, :, 256:384], constant=1.0)
        mnorm = self.SM("m_norm", l, 8)
        for h in range(4):
            for dc in range(2):
                ch = 2 * h + dc
                blk, rblk = self.wget("w_in", l, 0, OFF_MX + ch * 128, 128)
                self.proj_fm(blk, rblk, 0, hT, "h", [0, 1, 2, 3])
                pbs = [self.PB(i) for i in range(4)]
                for tb in range(NTB):
                    i = tb % 2
                    sl = slice(tb * TB, (tb + 1) * TB)
                    self.copy(mxh[:, dc, sl], self.bank(tb), [self.PB(tb)], [rmxh], eng="act")
                    self.conv_tb(A_all, pbs, cw, cb, ch, tb, xct[i], rxct[i], rsm)
                    self.act(xch[:, dc, sl], xct[i], AF.Silu, [rxct[i]], [rxch])
            blk, rblk = self.wget("w_in", l, 0, OFF_MO + h * 256, 256)
            for ec in range(2):
                banks = [0, 1, 2, 3] if ec == 0 else [4, 5, 6, 7]
                self.proj_fm(blk, rblk, ec * 128, hT, "h", banks)
                for tb in range(NTB):
                    self.act(smo[:, ec, tb * TB:(tb + 1) * TB], self.bank(banks[tb]), AF.Sigmoid, [self.PB(banks[tb])], [rsmo])
            for tb in range(NTB):
                t0 = tb * TB
                sl = slice(t0, t0 + TB)
                for dc in range(2):
                    ch = 2 * h + dc
                    self.mm(self.bank(0), self.bdw[:, 2, ch, :], xch[:, dc, sl], True, True, [rbd, rxch], [self.PB(0)])
                    self.copy(qTb[:, dc, :], self.bank(0), [self.PB(0)], [rqT], eng="act")
                    self.mm(self.bank(1), self.bdw[:, 3, ch, :], xch[:, dc, sl], True, True, [rbd, rxch], [self.PB(1)])
                    self.copy(kTb[:, dc, :], self.bank(1), [self.PB(1)], [rkT], eng="act")
                for ci in range(4):
                    c = tb * 4 + ci
                    ts_ = slice(t0 + ci * 128, t0 + (ci + 1) * 128)
                    for dc in range(2):
                        ch = 2 * h + dc
                        self.mm(self.bank(2)[:, dc * 128:(dc + 1) * 128], xch[:, dc, ts_], self.bdw[:, 3, ch, :], True, True,
                                [rbd, rxch], [self.PB(2)])
                        self.mm(self.bank(2)[:, 256 + dc * 128:256 + (dc + 1) * 128], mxh[:, dc, ts_],
                                self.bdw[:, 4, ch, :], True, True, [rbd, rmxh], [self.PB(2)])
                    self.ts(kw[:, ci, :], self.bank(2)[:, 0:256], wkt[:, c * 4 + h:c * 4 + h + 1], 1.0 / 16.0, ALU.mult, ALU.mult,
                            [self.PB(2), rwkt], [rkw])
                    self.copy(vx[:, ci, 0:256], self.bank(2)[:, 256:512], [self.PB(2)], [rvx], eng="act")
                ob = [3, 4, 5]
                for ci in range(4):
                    c = tb * 4 + ci
                    cs = slice(ci * 128, (ci + 1) * 128)
                    si = c % 2
                    bs = si
                    idx = h * 16 + c
                    for dc in range(2):
                        self.mm(self.bank(bs)[:, 0:128], kTb[:, dc, cs], qTb[:, dc, cs], dc == 0, dc == 1, [rkT, rqT], [self.PB(bs)])
                    if c < 15:
                        for dc in range(2):
                            self.mm(self.bank(6 + dc)[:, 0:384], kw[:, ci, dc * 128:(dc + 1) * 128], vx[:, ci, :], True, True,
                                    [rkw, rvx], [self.PB(6 + dc)])
                    pc, pn = c % 2, (c + 1) % 2
                    Cst, Cbf, rC, rCb = Cst2[pc], Cbf2[pc], rC2[pc], rCb2[pc]
                    if c > 0:
                        for dc in range(2):
                            self.act(Cbf[:, dc, :], Cst[:, dc, :], AF.Identity, [rC, rdecb], [rCb], scale=decb[:, idx:idx + 1])
                    self.stt(sT[si], self.bank(bs)[:, 0:128], wkt[:, c * 4 + h:c * 4 + h + 1], self.C("causal16"), ALU.mult, ALU.mult,
                             [self.PB(bs), rwkt, rc], [rsT[si]])
                    if c < 15:
                        for dc in range(2):
                            if c == 0:
                                self.copy(Cst2[pn][:, dc, :], self.bank(6 + dc)[:, 0:384], [self.PB(6 + dc)], [rC2[pn]])
                            else:
                                self.stt(Cst2[pn][:, dc, :], Cst[:, dc, :], decb[:, idx:idx + 1], self.bank(6 + dc)[:, 0:384],
                                         ALU.mult, ALU.add, [rC, rdecb, self.PB(6 + dc)], [rC2[pn]])
                    for ec in range(3):
                        es = slice(ec * 128, (ec + 1) * 128)
                        self.mm(self.bank(ob[ec])[:, cs], vx[:, ci, es], sT[si], True, c == 0, [rvx, rsT[si]], [self.PB(ob[ec])])
                        if c > 0:
                            for dc in range(2):
                                self.mm(self.bank(ob[ec])[:, cs], Cbf[:, dc, es], qTb[:, dc, cs], False, dc == 1, [rCb, rqT],
                                        [self.PB(ob[ec])])
                self.mm(self.bank(0), self.C("sel")[:, h * 128:(h + 1) * 128], bT[:, sl], True, True, [rb, rc], [self.PB(0)])
                self.act(thb, self.bank(0), AF.Exp, [self.PB(0)], [rthb], scale=-1.0)
                self.tt(rcp, self.bank(5), thb, ALU.max, [self.PB(5), rthb], [rrcp])
                self.stt(rcp, self.bank(5), -1.0, rcp, ALU.mult, ALU.max, [self.PB(5), rrcp], [rrcp])
                S.op("dve", "reciprocal", reads=[rrcp], writes=[rrcp], out=rcp, in_=rcp)
                for ec in range(2):
                    self.tt(hm[:, ec, :], self.bank(ob[ec]), rcp, ALU.mult, [self.PB(ob[ec]), rrcp], [rhm])
                    self.tt(hm[:, ec, :], hm[:, ec, :], smo[:, ec, sl], ALU.mult, [rhm, rsmo], [rhm])
                    self.act(sq[:, ec, :], hm[:, ec, :], AF.Square, [rhm], [rsq])
                for ec in range(2):
                    self.mm(self.bank(1), self.onesb[:, :], sq[:, ec, :], ec == 0, ec == 1, [rsq, self.R("onesb")], [self.PB(1)])
                self.rstd_from_bank(1, rs, 256.0, rrs)
                for ec in range(2):
                    ch = 2 * h + ec
                    self.stt(bo[:, ch, sl], hm[:, ec, :], mnorm[:, ch:ch + 1], rs, ALU.mult, ALU.mult, [rhm, rrs, rsm],
                             [self.R(("bo", tb))])

    def layer(self, l):
        S = self.S
        X, Y = self.X, self.Y
        xv = self.xview(X)
        hT = self.hview(self.RM)
        self.adaln(l, precomputed=(l > 0))
        self.ybump = 0
        tmps = self.norm_tmps(self.ytemp)
        self.norm(xv, "x", self.vecs[:, 0, :], self.modv[:, 0:8], hT, "h", tmps)
        toks = []
        for c in range(NCH):
            toks.append(S.dma("sp", self.d["xs"][:, c, :], xv[:, c, :], reads=[self.R(("x", c, tb)) for tb in range(NTB)],
                              writes=[self.R(("xsd", c))]))
        S.barrier()
        if self.stage == "h":
            return ("bf", hT)
        for bidx, (fn, wname) in enumerate(((self.ret_branch, "w_br_ret"), (self.lru_branch, "w_br_lru"), (self.mlstm_branch, "w_br_mlstm"))):
            if self.stage in ("bo0", "bo1", "bo2") and self.stage != "bo%d" % bidx:
                continue
            fn(l)
            S.barrier()
            self.ybump = 0
            if self.stage == "bo%d" % bidx:
                return ("bf", self.hview(X, 0))
            self.branch_merge(l, bidx, wname)
            S.barrier()
        if self.stage == "mg":
            return ("bf", self.hview(X, 16384))
        yv = self.xview(Y)
        for c in range(NCH):
            S.dma("sp", yv[:, c, :], self.d["xs"][:, c, :], reads=[self.R(("xsd", c))],
                  writes=[self.R(("xn", c, tb)) for tb in range(NTB)])
        mg = self.hview(X, 16384)
        for f in range(NCH):
            if f % 2 == 0:
                blk, rblk = self.wget("w_out", l, 0, f * 128, 256)
            banks = [0, 1, 2, 3] if f % 2 == 0 else [4, 5, 6, 7]
            self.proj_fm(blk, rblk, (f % 2) * 128, mg, "mg", banks)
            for tb in range(NTB):
                sl = slice(tb * TB, (tb + 1) * TB)
                self.stt(yv[:, f, sl], self.bank(banks[tb]), self.modv[:, 16 + f:17 + f], yv[:, f, sl], ALU.mult, ALU.add,
                         [self.PB(banks[tb]), self.R("vecs"), self.R(("xn", f, tb))], [self.R(("xn", f, tb))])
        S.barrier()
        if self.stage == "xmix":
            return ("f32", yv)
        save_Y = self.Y
        self.Y = X
        self.ybump = 0
        tmps = self.norm_tmps(self.ytemp)
        self.Y = save_Y
        self.norm(yv, "xn", self.vecs[:, 1, :], self.modv[:, 24:32], hT, "h2", tmps)
        S.barrier()
        hid = X[:, :].rearrange("p (j t) -> p j t", j=32)
        fsq = [self.fsq0[:, :], self.fsq1[:, :]]
        rfsq = [self.R("fsq0"), self.R("fsq1")]
        for half in range(2):
            for jb in range(16):
                blk, rblk = self.wget("w_ff1", l, 0, jb * 256, 256)
                for jc in range(2):
                    j = jb * 2 + jc
                    for t2 in range(2):
                        tb = half * 2 + t2
                        b = (j * 2 + t2) % 7
                        for k in range(NCH):
                            self.mm(self.bank(b), blk[:, k, jc * 128:(jc + 1) * 128], hT[:, k, tb * TB:(tb + 1) * TB], k == 0, k == NCH - 1,
                                    [rblk, self.R(("h2", tb))], [self.PB(b)])
                        i2 = (j * 2 + t2) % 2
                        self.act(fsq[i2], self.bank(b), AF.Square, [self.PB(b)], [rfsq[i2]])
                        self.stt(hid[:, j, t2 * TB:(t2 + 1) * TB], self.bank(b), 0.0, fsq[i2], ALU.is_gt, ALU.mult,
                                 [self.PB(b), rfsq[i2]], [self.R(("hid", j // 8, t2))])
                if l + 1 < self.nl and jb < 12:
                    self.adaln_block(l + 1, half * 12 + jb, 7, self.modn, self.R("modn"))
            for cb_ in range(4):
                banks = [0, 1, 2, 3] if cb_ % 2 == 0 else [4, 5, 6, 7]
                for kg in range(4):
                    blk, rblk = self.wget("w_ff2", l, kg, cb_ * 256, 256)
                    for fl in range(2):
                        for t2 in range(2):
                            b = banks[fl * 2 + t2]
                            for k in range(NCH):
                                self.mm(self.bank(b), blk[:, k, fl * 128:(fl + 1) * 128], hid[:, kg * 8 + k, t2 * TB:(t2 + 1) * TB],
                                        kg == 0 and k == 0, kg == 3 and k == NCH - 1, [rblk, self.R(("hid", kg, t2))], [self.PB(b)])
                for fl in range(2):
                    f = cb_ * 2 + fl
                    for t2 in range(2):
                        tb = half * 2 + t2
                        b = banks[fl * 2 + t2]
                        sl = slice(tb * TB, (tb + 1) * TB)
                        self.stt(yv[:, f, sl], self.bank(b), self.modv[:, 40 + f:41 + f], yv[:, f, sl], ALU.mult, ALU.add,
                                 [self.PB(b), self.R("vecs"), self.R(("xn", f, tb))], [self.R(("xn", f, tb))])
            S.barrier()
        self.X, self.Y = Y, X
        for c in range(NCH):
            for tb in range(NTB):
                self.res[("x", c, tb)] = self.res.pop(("xn", c, tb))
        if self.stage == "xffn":
            return ("f32", self.xview(self.X))
        return None

    def epilogue(self):
        S = self.S
        xv = self.xview(self.X)
        self.ybump = 0
        sq = [self.ytemp(TB, BF16) for _ in range(2)]
        rsq = [Res(), Res()]
        rs = self.ytemp(TB, F32)
        rrs = Res()
        yn = self.ytemp(NCH * TB, F32).rearrange("p (c t) -> p c t", c=NCH)
        ryn = Res()
        st = [self.ytemp(D, F32) for _ in range(2)]
        rst = [Res(), Res()]
        fn = self.SM("final_norm")
        ident = self.C("ident")
        toks = []
        for tb in range(NTB):
            sl = slice(tb * TB, (tb + 1) * TB)
            for c in range(NCH):
                i = c % 2
                self.act(sq[i], xv[:, c, sl], AF.Square, [self.R(("x", c, tb))], [rsq[i]])
                self.mm(self.bank(7), self.onesb[:, :], sq[i], c == 0, c == NCH - 1, [rsq[i], self.R("onesb")], [self.PB(7)])
            self.rstd_from_bank(7, rs, float(D), rrs)
            for c in range(NCH):
                self.stt(yn[:, c, :], xv[:, c, sl], fn[:, c:c + 1], rs, ALU.mult, ALU.mult, [self.R(("x", c, tb)), rrs, self.R("smalls")], [ryn])
            for ci in range(4):
                tt_ = tb * 4 + ci
                i = tt_ % 2
                for half in range(2):
                    b = (tt_ * 2 + half) % 6
                    for cc in range(4):
                        c = half * 4 + cc
                        S.op("pe", "transpose", reads=[ryn, self.R("consts")], writes=[self.PB(b)],
                             out=self.bank(b)[:, cc * 128:(cc + 1) * 128], in_=yn[:, c, ci * 128:(ci + 1) * 128], identity=ident)
                    self.copy(st[i][:, half * 512:(half + 1) * 512], self.bank(b), [self.PB(b)], [rst[i]], eng="act" if half else "dve")
                toks.append(S.dma("sp", self.d["out"][tt_ * 128:(tt_ + 1) * 128, :], st[i], reads=[rst[i]]))
        if not self.recording:
            S.wait_tokens("sp", toks)

    def build(self, nsmall, stage=None):
        self.nsmall = nsmall
        self.stage = stage
        self.setup()
        self.prologue()
        early = None
        for l in range(self.nl):
            early = self.layer(l)
            if early:
                break
        if early:
            kind, view = early
            S = self.S
            S.barrier()
            toks = []
            if kind == "f32":
                for c in range(NCH):
                    toks.append(S.dma("sp", self.dbg_out["dump"][:, c, :], view[:, c, :]))
            else:
                stg = [self.fsq0[:, :], self.fsq1[:, :]]
                rstg = [Res(), Res()]
                for c in range(NCH):
                    for tb in range(NTB):
                        i = tb % 2
                        sl = slice(tb * TB, (tb + 1) * TB)
                        self.copy(stg[i], view[:, c, sl], [], [rstg[i]])
                        toks.append(S.dma("sp", self.dbg_out["dump"][:, c, sl], stg[i], reads=[rstg[i]]))
            S.wait_tokens("sp", toks)
        else:
            self.epilogue()
        if not self.recording:
            self.S.emit()
        return self.nc


def make_program(nsmall, nlayers=L, debug=None, stage=None):
    g0 = Gen(nlayers, debug, record=None)
    g0.build(nsmall, stage)
    g = Gen(nlayers, debug, record=g0.wspecs)
    nc = g.build(nsmall, stage)
    return nc, g


def prepare_inputs(inputs):
    smalls = _build_smalls(inputs)
    bd = np.zeros((L, 8, 128, 8, 128), np.float32)
    bd[:, 0] = _blockdiag(inputs["lru_w_r"])
    bd[:, 1] = _blockdiag(inputs["lru_w_i"])
    bd[:, 2] = _blockdiag(inputs["m_w_q"])
    bd[:, 3] = _blockdiag(inputs["m_w_k"])
    bd[:, 4] = _blockdiag(inputs["m_w_v"])
    bd[:, 5] = _blockdiag(inputs["m_w_q"], transpose=True)
    bd[:, 6] = _blockdiag(inputs["m_w_k"], transpose=True)
    bd[:, 7] = _blockdiag(inputs["m_w_v"], transpose=True)
    shared = {"consts": CONSTS, "smalls": smalls, "bd": bd}
    for n in ("w_ada", "w_in", "w_br_ret", "w_br_lru", "w_br_mlstm", "w_out", "w_ff1", "w_ff2"):
        shared[n] = np.ascontiguousarray(np.asarray(inputs[n], np.float32))
    in_maps = []
    x = np.asarray(inputs["x"], np.float32)
    c = np.asarray(inputs["c"], np.float32)
    pos = np.asarray(inputs["positions"], np.int32)
    for b in range(8):
        m = dict(shared)
        m["x"] = np.ascontiguousarray(x[b])
        m["cvec"] = np.ascontiguousarray(c[b].reshape(8, 128).T)
        m["pos"] = np.ascontiguousarray(pos[b])
        in_maps.append(m)
    return in_maps, smalls.shape[1]


def kernel(**inputs):
    in_maps, nsmall = prepare_inputs(inputs)
    nc, _ = make_program(nsmall)
    res = run_bass_kernel_spmd(nc, in_maps, core_ids=list(range(8)))
    out = np.stack([np.asarray(r["out"], np.float32) for r in res.results], axis=0)
    return out
```
